# Optimizing a Trainium2 kernel written in Bass

```python
import jax, jax.numpy as jnp
from jax import lax
import numpy as np

D_MODEL = 1024
BATCH = 32
SEQ = 2048
DEPTH = 1

POOL_GROUPS = 4
POOL_WINDOWS = (2, 4, 8, 16)
POOL_GROUP_W = 128
POOL_W = POOL_GROUPS * POOL_GROUP_W
POOL_OUT_GROUP_W = D_MODEL // POOL_GROUPS
ATTN_HEADS = 16
HEAD_DIM = 64
ATTN_W = ATTN_HEADS * HEAD_DIM
Q_BLOCK = 128
N_BRANCH = 2
IN_W = POOL_W + 3 * ATTN_W + ATTN_HEADS + N_BRANCH * D_MODEL
PEER_HEADS = 8
N_KEYS = 128
N_EXPERTS = N_KEYS * N_KEYS
PEER_HALF = 128
PEER_KEY_DIM = 2 * PEER_HALF
PEER_TOPK = 16
TOK_CHUNK = 128
N_MOD = 6
EPS = 1e-6
NEG_INF = -1e30

kernel_name = "hybrid_pool_fox_peer_adaln_block"


def rmsnorm(x, g):
    xf = x.astype(jnp.float32)
    y = xf * lax.rsqrt(jnp.mean(xf * xf, axis=-1, keepdims=True) + EPS)
    return (y * g.astype(jnp.float32)).astype(x.dtype)


def modulate(h, shift, scale):
    return h * (1.0 + scale[:, None, :]) + shift[:, None, :]


def causal_pool_mixer(u, w_pool, pool_scale):
    B, S, _ = u.shape
    ug = u.reshape(B, S, POOL_GROUPS, POOL_GROUP_W)
    cs = jnp.cumsum(ug.astype(jnp.float32), axis=1)
    pos = jnp.arange(S)
    means = []
    for g, w in enumerate(POOL_WINDOWS):
        csg = cs[:, :, g]
        lag = jnp.pad(csg, ((0, 0), (w, 0), (0, 0)))[:, :S]
        cnt = jnp.minimum(pos + 1, w).astype(jnp.float32)[None, :, None]
        means.append((csg - lag) / cnt)
    pooled = jnp.stack(means, axis=2).astype(u.dtype) - ug
    y = jnp.einsum('bsgc,gcd->bsgd', pooled, w_pool).reshape(B, S, D_MODEL)
    return y * pool_scale


def forgetting_attention(q, k, v, log_f):
    B, S, H, Dh = q.shape
    F = jnp.transpose(jnp.cumsum(log_f.astype(jnp.float32), axis=1), (0, 2, 1))
    scale = Dh ** -0.5
    outs = []
    for i in range(S // Q_BLOCK):
        q0 = i * Q_BLOCK
        L = q0 + Q_BLOCK
        s = jnp.einsum('bqhd,bkhd->bhqk', q[:, q0:L], k[:, :L],
                       preferred_element_type=jnp.float32) * scale
        s = s + F[:, :, q0:L, None] - F[:, :, None, :L]
        qpos = q0 + jnp.arange(Q_BLOCK)
        kpos = jnp.arange(L)
        s = jnp.where(kpos[None, :] <= qpos[:, None], s, NEG_INF)
        p = jax.nn.softmax(s, axis=-1).astype(v.dtype)
        outs.append(jnp.einsum('bhqk,bkhd->bqhd', p, v[:, :L]))
    return jnp.concatenate(outs, axis=1).reshape(B, S, H * Dh)


def peer_ffn(h, w_query, sub_keys, expert_u, expert_v):
    B, S, D = h.shape
    q = jnp.einsum('bsd,dk->bsk', h, w_query).reshape(B, S, PEER_HEADS, 2, PEER_HALF)
    sc = jnp.einsum('bshpc,hpnc->bshpn', q, sub_keys,
                    preferred_element_type=jnp.float32)
    s1, i1 = lax.top_k(sc[..., 0, :], PEER_TOPK)
    s2, i2 = lax.top_k(sc[..., 1, :], PEER_TOPK)
    cand_s = (s1[..., :, None] + s2[..., None, :]).reshape(B, S, PEER_HEADS, PEER_TOPK * PEER_TOPK)
    cand_i = (i1[..., :, None] * N_KEYS + i2[..., None, :]).reshape(B, S, PEER_HEADS, PEER_TOPK * PEER_TOPK)
    top_s, sel = lax.top_k(cand_s, PEER_TOPK)
    idx = jnp.take_along_axis(cand_i, sel, axis=-1)
    gates = jax.nn.softmax(top_s, axis=-1).astype(h.dtype)
    T = B * S
    n_chunks = T // TOK_CHUNK
    hc = h.reshape(n_chunks, TOK_CHUNK, D)
    ic = idx.reshape(n_chunks, TOK_CHUNK, PEER_HEADS, PEER_TOPK)
    gc = gates.reshape(n_chunks, TOK_CHUNK, PEER_HEADS, PEER_TOPK)

    def chunk(args):
        xc, ec, wc = args
        u = expert_u[ec]
        a = jax.nn.gelu(jnp.einsum('cd,chkd->chk', xc, u), approximate=False) * wc
        return jnp.einsum('chk,chkd->cd', a, expert_v[ec])

    out = lax.map(chunk, (hc, ic, gc))
    return out.reshape(B, S, D)


def setup_inputs(seed: int = 0) -> dict:
    key = jax.random.key(seed)
    ks = jax.random.split(key, 20)
    nrm = jax.random.normal
    d = D_MODEL
    x = nrm(ks[0], (BATCH, SEQ, d), jnp.float32)
    c = nrm(ks[1], (BATCH, d), jnp.float32)
    w_mod = nrm(ks[2], (DEPTH, d, N_MOD * d), jnp.float32) * (0.5 * d ** -0.5)
    b_mod = nrm(ks[3], (DEPTH, N_MOD * d), jnp.float32) * 0.02
    norm1_g = 1.0 + 0.02 * nrm(ks[4], (DEPTH, d), jnp.float32)
    w_in = nrm(ks[5], (DEPTH, d, IN_W), jnp.float32) * d ** -0.5
    b_f = jax.random.uniform(ks[6], (DEPTH, ATTN_HEADS), jnp.float32, 1.0, 4.0)
    w_pool = nrm(ks[7], (DEPTH, POOL_GROUPS, POOL_GROUP_W, POOL_OUT_GROUP_W), jnp.float32) * POOL_GROUP_W ** -0.5
    pool_scale = 1.0 + 0.02 * nrm(ks[8], (DEPTH, d), jnp.float32)
    w_out = nrm(ks[9], (DEPTH, d, d), jnp.float32) * d ** -0.5
    norm2_g = 1.0 + 0.02 * nrm(ks[10], (DEPTH, d), jnp.float32)
    peer_w_query = nrm(ks[11], (DEPTH, d, PEER_HEADS * PEER_KEY_DIM), jnp.float32) * d ** -0.5
    peer_sub_keys = nrm(ks[12], (DEPTH, PEER_HEADS, 2, N_KEYS, PEER_HALF), jnp.float32) * PEER_HALF ** -0.5
    peer_u = nrm(ks[13], (DEPTH, N_EXPERTS, d), jnp.float32) * d ** -0.5
    peer_v = nrm(ks[14], (DEPTH, N_EXPERTS, d), jnp.float32) * PEER_HEADS ** -0.5
    final_g = 1.0 + 0.02 * nrm(ks[15], (d,), jnp.float32)
    return {"x": x, "c": c, "w_mod": w_mod, "b_mod": b_mod, "norm1_g": norm1_g,
            "w_in": w_in, "b_f": b_f, "w_pool": w_pool, "pool_scale": pool_scale,
            "w_out": w_out, "norm2_g": norm2_g, "peer_w_query": peer_w_query,
            "peer_sub_keys": peer_sub_keys, "peer_u": peer_u, "peer_v": peer_v,
            "final_g": final_g}


def reference(x, c, w_mod, b_mod, norm1_g, w_in, b_f, w_pool, pool_scale, w_out,
              norm2_g, peer_w_query, peer_sub_keys, peer_u, peer_v, final_g):
    B, S, D = x.shape
    for l in range(DEPTH):
        mod = jnp.einsum('bd,de->be', c, w_mod[l]) + b_mod[l]
        sh1, sc1, gt1, sh2, sc2, gt2 = jnp.split(mod, N_MOD, axis=-1)

        h = modulate(rmsnorm(x, norm1_g[l]), sh1, sc1)
        z = jnp.einsum('bsd,de->bse', h, w_in[l])
        o = 0
        u_pool = z[..., o:o + POOL_W]; o += POOL_W
        q = z[..., o:o + ATTN_W].reshape(B, S, ATTN_HEADS, HEAD_DIM); o += ATTN_W
        k = z[..., o:o + ATTN_W].reshape(B, S, ATTN_HEADS, HEAD_DIM); o += ATTN_W
        v = z[..., o:o + ATTN_W].reshape(B, S, ATTN_HEADS, HEAD_DIM); o += ATTN_W
        f_logit = z[..., o:o + ATTN_HEADS]; o += ATTN_HEADS
        g_a = jax.nn.sigmoid(z[..., o:o + D]); o += D
        g_b = jax.nn.sigmoid(z[..., o:o + D])

        y_a = causal_pool_mixer(u_pool, w_pool[l], pool_scale[l])
        log_f = jax.nn.log_sigmoid((f_logit + b_f[l]).astype(jnp.float32))
        y_b = forgetting_attention(q, k, v, log_f)
        y = g_a * y_a + g_b * y_b
        x = x + gt1[:, None, :] * jnp.einsum('bsd,de->bse', y, w_out[l])

        h2 = modulate(rmsnorm(x, norm2_g[l]), sh2, sc2)
        x = x + gt2[:, None, :] * peer_ffn(h2, peer_w_query[l], peer_sub_keys[l], peer_u[l], peer_v[l])
    return rmsnorm(x, final_g)
```

```python
import numpy as np
from contextlib import ExitStack
import concourse.bass as bass
import concourse.mybir as mybir
from concourse.bass_utils import run_bass_kernel_spmd

F32 = mybir.dt.float32
BF16 = mybir.dt.bfloat16
U32 = mybir.dt.uint32
U8 = mybir.dt.uint8
AF = mybir.ActivationFunctionType
ALU = mybir.AluOpType
AX = mybir.AxisListType

D = 1024
NCORES = 8
EPS = 1e-6
ENGS = ("pe", "act", "dve", "pool", "sp")
ISZ = {F32: 4, BF16: 2, U32: 4, U8: 1}


class Instr:
    __slots__ = ("eng", "idx", "fn", "args", "kwargs", "dma", "deps", "signal",
                 "count", "dsem", "dval")

    def __init__(self, eng, idx, fn, args, kwargs, dma):
        self.eng = eng
        self.idx = idx
        self.fn = fn
        self.args = args
        self.kwargs = kwargs
        self.dma = dma
        self.deps = []
        self.signal = False
        self.count = 0
        self.dsem = None
        self.dval = 0


class Prog:
    def __init__(self, nc, n_dma_sems=40, n_sw=12):
        self.nc = nc
        self.streams = {e: [] for e in ENGS}
        self.reg = {}
        self.barrier = {e: {} for e in ENGS}
        self.n_dma_sems = n_dma_sems
        self.n_sw = n_sw
        self.sw_count = 0
        self.hw_count = 0
        self.dma_last = [None] * n_dma_sems
        self.dma_uses = [0] * n_dma_sems

    def op(self, eng, fn, *args, reads=(), writes=(), dma=False, **kwargs):
        st = self.streams[eng]
        ins = Instr(eng, len(st), fn, args, kwargs, dma)
        deps = {}

        def add(p):
            if p.dma:
                deps[("d", id(p))] = p
            else:
                k = ("e", p.eng)
                q = deps.get(k)
                if q is None or q.idx < p.idx:
                    deps[k] = p

        for r in reads:
            s = self.reg.get(r)
            if s is not None and s[0] is not None:
                add(s[0])
        for w in writes:
            s = self.reg.get(w)
            if s is None:
                continue
            wr = s[0]
            if wr is not None and (wr.dma or dma or wr.eng != eng):
                add(wr)
            for e2, rd in s[1].items():
                if rd.dma or dma or rd.eng != eng:
                    add(rd)
            for rd in s[2]:
                add(rd)
        for p in self.barrier[eng].values():
            if p is not ins:
                add(p)
        self.barrier[eng] = {}
        if dma:
            if eng == "pool":
                slot = self.sw_count % self.n_sw
                self.sw_count += 1
            else:
                slot = self.n_sw + self.hw_count % (self.n_dma_sems - self.n_sw)
                self.hw_count += 1
            prev = self.dma_last[slot]
            if prev is not None:
                deps[("d", id(prev))] = prev
            self.dma_uses[slot] += 1
            ins.dsem = slot
            ins.dval = 16 * self.dma_uses[slot]
            self.dma_last[slot] = ins
        final = []
        for p in deps.values():
            if (not p.dma) and (not dma) and p.eng == eng and eng == "pe":
                continue
            p.signal = True
            final.append(p)
        ins.deps = final
        for r in reads:
            s = self.reg.get(r)
            if s is None:
                s = self.reg[r] = [None, {}, []]
            if dma:
                s[2].append(ins)
            else:
                s[1][eng] = ins
        for w in writes:
            self.reg[w] = [ins, {}, []]
        st.append(ins)
        return ins

    def barrier_all(self, include_sw=False):
        last = {}
        for e in ENGS:
            for q in reversed(self.streams[e]):
                if not q.dma and q.fn is not None:
                    last[e] = q
                    break
        dmas = [d for i_, d in enumerate(self.dma_last) if d is not None and (include_sw or i_ >= self.n_sw)]
        for e in ENGS:
            b = {}
            for e2, p in last.items():
                b[("e", e2)] = p
            for d in dmas:
                b[("d", id(d))] = d
            self.barrier[e] = b

    def emit(self, es):
        nc = self.nc
        self.barrier_all(include_sw=True)
        self.op("sp", None)
        for e in ENGS:
            c = 0
            for ins in self.streams[e]:
                if ins.dma:
                    continue
                if ins.signal:
                    c += 1
                    ins.count = c
        esems = {e: es.enter_context(nc.semaphore("s_" + e)) for e in ENGS}
        dsems = [es.enter_context(nc.semaphore("d%d" % i)) for i in range(self.n_dma_sems)]
        block = es.enter_context(nc.Block())
        handles = {"pe": block.tensor, "act": block.scalar, "dve": block.vector,
                   "pool": block.gpsimd, "sp": block.sync}

        def make(e):
            def body(eng):
                waited = {}
                for ins in self.streams[e]:
                    for p in ins.deps:
                        if p.dma:
                            sem, val, key = dsems[p.dsem], p.dval, ("d", p.dsem)
                        else:
                            sem, val, key = esems[p.eng], p.count, ("e", p.eng)
                        if waited.get(key, 0) >= val:
                            continue
                        waited[key] = val
                        eng.wait_ge(sem, val)
                    if ins.fn is None:
                        continue
                    r = getattr(eng, ins.fn)(*ins.args, **ins.kwargs)
                    if ins.dma:
                        r.then_inc(dsems[ins.dsem], 16)
                    elif ins.signal:
                        r.then_inc(esems[e], 1)
            return body

        for e in ENGS:
            if self.streams[e]:
                handles[e](make(e))


NCONST = 128 + 128 + 1536 + 128 + 8 + 256 + 2048 + 128
C_ID, C_TRI, C_BAND, C_IOTA, C_HM, C_EO, C_SEL, C_NEG = 0, 128, 256, 1792, 1920, 1928, 2184, 4232


def make_consts():
    c = np.zeros((128, NCONST), np.float32)
    c[:, C_ID:C_ID + 128] = np.eye(128)
    s = np.arange(128)
    c[:, C_TRI:C_TRI + 128] = (s[:, None] <= s[None, :])
    band = np.zeros((128, 4, 3, 128), np.float32)
    for g, w in enumerate((2, 4, 8, 16)):
        for t in range(128):
            for ss in range(max(0, t - w + 1), t + 1):
                band[ss, g, 0, t] += 1.0 / w
            band[t, g, 0, t] -= 1.0
            for sp in range(max(0, 128 + t - w + 1), 128):
                band[sp, g, 1, t] += 1.0 / w
            cnt = min(t + 1, w)
            for ss in range(max(0, t - w + 1), t + 1):
                band[ss, g, 2, t] += 1.0 / cnt
            band[t, g, 2, t] -= 1.0
    c[:, C_BAND:C_BAND + 1536] = band.reshape(128, 1536)
    c[:, C_IOTA:C_IOTA + 128] = s[None, :]
    c[:, C_HM:C_HM + 8] = (s[:, None] // 16 == np.arange(8)[None, :])
    eo = np.zeros((128, 2, 128), np.float32)
    eo[:, 0, 0:64] = 1.0
    eo[:, 1, 64:128] = 1.0
    c[:, C_EO:C_EO + 256] = eo.reshape(128, 256)
    sel = np.zeros((128, 16, 128), np.float32)
    for r in range(32):
        sel[r, r % 16, :] = 1.0
    c[:, C_SEL:C_SEL + 2048] = sel.reshape(128, 2048)
    c[:, C_NEG:C_NEG + 128] = np.where(s[:, None] > s[None, :], -30000.0, 0.0)
    return c


def build_nc(S, NSEQ, debug=False):
    NB = S // 128
    NT = S // 256
    PIECE = min(512, S)
    QH = min(1024, S)
    NQH = S // QH
    NPC = QH // PIECE
    TCH = min(512, S)
    NTC = S // TCH

    nc = bass.Bass("TRN2", target_bir_lowering=False)
    dt = nc.dram_tensor

    def din(name, shape, dtype=F32):
        return dt(name, list(shape), dtype, kind="ExternalInput").ap()

    def dscr(name, shape, dtype=BF16):
        return dt(name, list(shape), dtype, kind="Internal").ap()

    x_d = din("x", [NSEQ, S, D])
    cT_d = din("cT", [128, 8 * NSEQ])
    wmod_d = din("wmod", [6, 128, 8 * 1024])
    bmT_d = din("bmT", [128, 48])
    vecT_d = din("vecT", [128, 24])
    fg_d = din("fg", [1, 1024])
    bf_d = din("bf", [1, 16])
    wpairs_d = din("wpairs", [8, 128, 5 * 1024])
    wu_d = din("wu", [128, 4 * 1024])
    wf_d = din("wf", [128, 128])
    wout_d = din("wout", [128, 8 * 1024])
    wq_d = din("wq", [4, 128, 4 * 1024])
    keysT_d = din("keysT", [128, 2048])
    wpool_d = din("wpool", [128, 1024])
    UT_d = din("UT", [128, 128, 1024])
    V_d = din("Vp", [128, 128, 1024])
    consts_d = din("consts", [128, NCONST])
    out_d = dt("out", [NSEQ, S, D], F32, kind="ExternalOutput").ap()

    wpairs_b = dscr("wpairs_b", [8, 128, 5 * 1024])
    wu_b = dscr("wu_b", [128, 4 * 1024])
    wf_b = dscr("wf_b", [128, 128])
    wout_b = dscr("wout_b", [128, 8 * 1024])
    wq_b = dscr("wq_b", [4, 128, 4 * 1024])
    keysT_b = dscr("keysT_b", [128, 2048])
    wpool_b = dscr("wpool_b", [128, 1024])
    UT_b = dscr("UT_b", [128, 128, 1024])
    V_b = dscr("V_b", [128, 128, 1024])

    dbg = {}
    es = ExitStack()
    ARENA = 206 * 1024
    arena = es.enter_context(nc.sbuf_tensor("arena", [128, ARENA], U8))
    banks = [es.enter_context(nc.psum_tensor("psb%d" % i, [128, 512], F32)) for i in range(8)]
    PS = [b[:] for b in banks]
    PSB = [b[:].bitcast(BF16) for b in banks]
    PK = ["ps%d" % i for i in range(8)]
    P = Prog(nc)
    top = [0]

    def alloc(shape, dtype, parts=128):
        n = int(np.prod(shape[1:])) * ISZ[dtype]
        off = (top[0] + 63) // 64 * 64
        top[0] = off + n
        assert top[0] <= ARENA, ("arena overflow", top[0])
        v = arena[:, off:off + n].bitcast(dtype)
        if shape[0] < 128:
            v = v[0:shape[0]]
        if len(shape) == 3:
            v = v.rearrange("p (a b) -> p a b", a=shape[1])
        elif len(shape) == 4:
            v = v.rearrange("p (a b c) -> p a b c", a=shape[1], b=shape[2])
        elif len(shape) == 5:
            v = v.rearrange("p (a b c d) -> p a b c d", a=shape[1], b=shape[2], c=shape[3])
        return v

    def dma(eng, out, in_, reads=(), writes=()):
        return P.op(eng, "dma_start", out=out, in_=in_, reads=reads, writes=writes, dma=True)

    def mm(out, lhsT, rhs, start, stop, reads, writes):
        return P.op("pe", "matmul", out, lhsT=lhsT, rhs=rhs, start=start, stop=stop,
                    reads=reads, writes=writes)

    def tr(out, in_, ident, reads, writes):
        return P.op("pe", "transpose", out=out, in_=in_, identity=ident, reads=reads, writes=writes)

    def act(out, in_, func, reads, writes, **kw):
        return P.op("act", "activation", out=out, in_=in_, func=func, reads=reads, writes=writes, **kw)

    def tt(eng, out, in0, in1, op, reads, writes):
        return P.op(eng, "tensor_tensor", out=out, in0=in0, in1=in1, op=op, reads=reads, writes=writes)

    def ts(eng, out, in0, s1, s2, op0, op1, reads, writes):
        if s2 is None:
            return P.op(eng, "tensor_scalar", out=out, in0=in0, scalar1=s1, scalar2=None, op0=op0,
                        reads=reads, writes=writes)
        return P.op(eng, "tensor_scalar", out=out, in0=in0, scalar1=s1, scalar2=s2, op0=op0, op1=op1,
                    reads=reads, writes=writes)

    def cp(eng, out, in_, reads, writes):
        return P.op(eng, "tensor_copy", out=out, in_=in_, reads=reads, writes=writes)

    def memset(eng, ap, val, writes):
        return P.op(eng, "memset", ap, val, writes=writes)

    ident_bf = alloc([128, 128], BF16)
    ident_f = alloc([128, 128], F32)
    ones_f = alloc([128, 128], F32)
    tri_f = alloc([128, 128], F32)
    tri_bf = alloc([128, 128], BF16)
    neg_bf = alloc([128, 128], BF16)
    band_bf = alloc([128, 4, 3, 128], BF16)
    iota_bf = alloc([128, 128], BF16)
    hmask_bf = alloc([128, 8], BF16)
    keysT_sb = alloc([128, 16, 128], BF16)
    wpool_sb = alloc([128, 4, 256], BF16)
    fg_bc = alloc([128, 1024], F32)
    bf_bc = alloc([128, 16], F32)
    modT = alloc([128, 48, NSEQ], F32)
    vecT = alloc([128, 3, 8], F32)
    bmT = alloc([128, 48], F32)
    cT = alloc([128, 8, NSEQ], F32)
    gs1T = alloc([128, 8, NSEQ], F32)
    gs2T = alloc([128, 8, NSEQ], F32)
    gt1_bc = alloc([128, 1024], F32)
    gt2_bc = alloc([128, 1024], F32)
    yT = alloc([128, 8, S], BF16)
    small = alloc([128, 32], F32)
    persist_top = top[0]

    cst = alloc([128, NCONST], F32)
    dma("sp", cst, consts_d, writes=["cst"])
    dma("sp", vecT, vecT_d.rearrange("p (a b) -> p a b", a=3), writes=["vecT"])
    dma("sp", bmT, bmT_d, writes=["bmT"])
    dma("sp", cT, cT_d.rearrange("p (a b) -> p a b", a=8), writes=["cT"])
    dma("sp", fg_bc, fg_d.partition_broadcast(128), writes=["fg_bc"])
    dma("sp", bf_bc, bf_d.partition_broadcast(128), writes=["bf_bc"])
    dma("pool", wu_b, wu_d, writes=["wu_b"])
    dma("pool", wf_b, wf_d, writes=["wf_b"])
    dma("pool", keysT_b, keysT_d, writes=["keysT_b"])
    dma("pool", wpool_b, wpool_d, writes=["wpool_b"])
    for p in range(8):
        dma("pool", wpairs_b[p], wpairs_d[p], writes=[("wpairs_b", p)])
    dma("pool", wout_b, wout_d, writes=["wout_b"])
    for i in range(4):
        dma("pool", wq_b[i], wq_d[i], writes=[("wq_b", i)])

    cp("dve", ident_bf, cst[:, C_ID:C_ID + 128], ["cst"], ["ident_bf"])
    cp("dve", ident_f, cst[:, C_ID:C_ID + 128], ["cst"], ["ident_f"])
    memset("dve", ones_f, 1.0, ["ones_f"])
    cp("dve", tri_f, cst[:, C_TRI:C_TRI + 128], ["cst"], ["tri_f"])
    cp("dve", tri_bf, cst[:, C_TRI:C_TRI + 128], ["cst"], ["tri_bf"])
    cp("dve", neg_bf, cst[:, C_NEG:C_NEG + 128], ["cst"], ["neg_bf"])
    cp("dve", band_bf.rearrange("p a b c -> p (a b c)"), cst[:, C_BAND:C_BAND + 1536], ["cst"], ["band_bf"])
    cp("dve", iota_bf, cst[:, C_IOTA:C_IOTA + 128], ["cst"], ["iota_bf"])
    cp("dve", hmask_bf, cst[:, C_HM:C_HM + 8], ["cst"], ["hmask_bf"])
    dma("sp", keysT_sb.rearrange("p a b -> p (a b)"), keysT_b, reads=["keysT_b"], writes=["keysT_sb"])
    dma("sp", wpool_sb.rearrange("p a b -> p (a b)"), wpool_b, reads=["wpool_b"], writes=["wpool_sb"])

    wm = [alloc([128, 8, 1024], F32), alloc([128, 8, 1024], F32)]
    for part in range(6):
        w = wm[part % 2]
        wk_ = ("wm", part % 2)
        dma("sp", w.rearrange("p a b -> p (a b)"), wmod_d[part], writes=[wk_])
        pb = 0
        for kc in range(8):
            for dk in range(8):
                mm(PS[pb][:, kc * NSEQ:(kc + 1) * NSEQ], w[:, dk, kc * 128:(kc + 1) * 128], cT[:, dk, :],
                   dk == 0, dk == 7, [wk_, "cT"], [PK[pb]])
        tt("dve", modT[:, part * 8:(part + 1) * 8, :],
           PS[pb][:, 0:8 * NSEQ].rearrange("p (a b) -> p a b", a=8),
           bmT[:, part * 8:(part + 1) * 8].unsqueeze(2).to_broadcast([128, 8, NSEQ]), ALU.add,
           ["bmT"], ["modT", PK[pb]])
    for (gs, gi, sc0) in ((gs1T, 0, 8), (gs2T, 1, 32)):
        ts("dve", gs, modT[:, sc0:sc0 + 8, :], 1.0, None, ALU.add, None, ["modT"], ["gs%d" % gi])
        tt("dve", gs, gs, vecT[:, gi, :].unsqueeze(2).to_broadcast([128, 8, NSEQ]), ALU.mult,
           ["gs%d" % gi, "vecT"], ["gs%d" % gi])
    memset("dve", small, 0.0, ["small"])
    top[0] = persist_top
    P.barrier_all()

    def bcast_gate(b, j0, dst, dkey):
        dg = alloc([128, 8, 128], F32)
        for k in range(8):
            ts("dve", dg[:, k, :], ident_f, modT[:, j0 + k, b:b + 1], None, ALU.mult, None,
               ["ident_f", "modT"], ["dg"])
        for k in range(8):
            pb = 6 + k // 4
            mm(PS[pb][:, (k % 4) * 128:(k % 4 + 1) * 128], ones_f, dg[:, k, :], True, True,
               ["ones_f", "dg"], [PK[pb]])
        cp("dve", dst[:, 0:512], PS[6], [], [dkey, PK[6]])
        cp("dve", dst[:, 512:1024], PS[7], [], [dkey, PK[7]])

    def rstd_of(src, reads, col, junk=None, jkey="junk"):
        if junk is None:
            junk = alloc([128, 1024], BF16)
        memset("dve", small[:, col:col + 1], 0.0, ["small"])
        act(junk, src, AF.Square, reads + ["small"], ["small", jkey], accum_out=small[:, col:col + 1])
        act(small[:, col + 2:col + 3], small[:, col:col + 1], AF.Ln, ["small"], ["small"],
            scale=1.0 / D, bias=EPS)
        act(small[:, col + 1:col + 2], small[:, col + 2:col + 3], AF.Exp, ["small"], ["small"], scale=-0.5)
        return small[:, col + 1:col + 2]

    def norm_to_T(src, skey, dstT, dkey, t0, gsT, shT, b, xs_b, xkey, pb):
        m0 = top[0]
        r = rstd_of(src, [skey], 0)
        ts("dve", xs_b, src, r, None, ALU.mult, None, [skey, "small"], [xkey])
        for k in range(8):
            tr(PSB[pb][:, k * 128:(k + 1) * 128], xs_b[:, k * 128:(k + 1) * 128], ident_bf,
               [xkey, "ident_bf"], [PK[pb]])
        for k in range(8):
            o = dstT[:, k, t0:t0 + 128]
            i = PSB[pb][:, k * 128:(k + 1) * 128]
            if k % 2 == 0:
                act(o, i, AF.Identity, ["gsT", "modT"], [dkey, PK[pb]], scale=gsT[:, k, b:b + 1],
                    bias=shT[:, k, b:b + 1])
            else:
                ts("dve", o, i, gsT[:, k, b:b + 1], shT[:, k, b:b + 1], ALU.mult, ALU.add,
                   ["gsT", "modT"], [dkey, PK[pb]])
        top[0] = m0

    def norm_to_T_multi(blks, dstT, dkey, gsT, shT, b, junk=None, jkey="junk"):
        m0 = top[0]
        if junk is None:
            junk = alloc([128, 1024], BF16)
        n = len(blks)
        for i, (src, skey, t0, xs_b, xkey, pb) in enumerate(blks):
            c0 = 8 + 3 * i
            memset("dve", small[:, c0:c0 + 1], 0.0, [("small", i)])
        for i, (src, skey, t0, xs_b, xkey, pb) in enumerate(blks):
            c0 = 8 + 3 * i
            act(junk, src, AF.Square, [skey, ("small", i)], [("small", i), jkey], accum_out=small[:, c0:c0 + 1])
        for i in range(n):
            c0 = 8 + 3 * i
            act(small[:, c0 + 2:c0 + 3], small[:, c0:c0 + 1], AF.Ln, [("small", i)], [("small", i)],
                scale=1.0 / D, bias=EPS)
        for i in range(n):
            c0 = 8 + 3 * i
            act(small[:, c0 + 1:c0 + 2], small[:, c0 + 2:c0 + 3], AF.Exp, [("small", i)], [("small", i)], scale=-0.5)
        for i, (src, skey, t0, xs_b, xkey, pb) in enumerate(blks):
            c0 = 8 + 3 * i
            ts("dve", xs_b, src, small[:, c0 + 1:c0 + 2], None, ALU.mult, None, [skey, ("small", i)], [xkey])
        for i, (src, skey, t0, xs_b, xkey, pb) in enumerate(blks):
            for k in range(8):
                tr(PSB[pb][:, k * 128:(k + 1) * 128], xs_b[:, k * 128:(k + 1) * 128], ident_bf,
                   [xkey, "ident_bf"], [PK[pb]])
        for i, (src, skey, t0, xs_b, xkey, pb) in enumerate(blks):
            for k in range(8):
                o = dstT[:, k, t0:t0 + 128]
                ii = PSB[pb][:, k * 128:(k + 1) * 128]
                if k % 2 == 0:
                    act(o, ii, AF.Identity, ["gsT", "modT"], [dkey, PK[pb]], scale=gsT[:, k, b:b + 1],
                        bias=shT[:, k, b:b + 1])
                else:
                    ts("dve", o, ii, gsT[:, k, b:b + 1], shT[:, k, b:b + 1], ALU.mult, ALU.add,
                       ["gsT", "modT"], [dkey, PK[pb]])
        top[0] = m0

    for b in range(NSEQ):
        top[0] = persist_top
        P.barrier_all()
        bcast_gate(b, 16, gt1_bc, "gt1_bc")
        bcast_gate(b, 40, gt2_bc, "gt2_bc")
        top[0] = persist_top
        P.barrier_all()
        hT = alloc([128, 8, S], BF16)
        pooledT = alloc([128, 4, S], BF16)
        wpair = [alloc([128, 5, 8, 128], BF16), alloc([128, 5, 8, 128], BF16)]
        Lf = alloc([128, NB, 16], F32)
        Tot = alloc([128, NB, 16], F32)
        carry = alloc([128, NB, 16], F32)
        Csb = alloc([128, NB, 16], F32)
        negCf = alloc([128, NB, 16], F32)
        hif = alloc([128, NB, 16], F32)
        Chl = alloc([128, NB, 32], BF16)
        negC = alloc([32, S], BF16)
        a_top = top[0]
        NXS = min(4, NB)
        xsf = [alloc([128, 1024], F32) for _ in range(NXS)]
        xsb = [alloc([128, 1024], BF16) for _ in range(NXS)]
        u_tm = alloc([128, NB, 512], BF16)
        wu_sb = alloc([128, 4, 8, 128], BF16)
        wf_sb = alloc([128, 8, 16], BF16)
        dma("sp", wu_sb.rearrange("p a b c -> p (a b c)"), wu_b, reads=["wu_b"], writes=["wu_sb"])
        dma("sp", wf_sb.rearrange("p a b -> p (a b)"), wf_b, reads=["wf_b"], writes=["wf_sb"])
        dma("sp", wpair[0].rearrange("p a b c -> p (a b c)"), wpairs_b[0], reads=[("wpairs_b", 0)],
            writes=[("wpair", 0)])
        for j0 in range(0, NB, NXS):
            blks = []
            for sl in range(NXS):
                j = j0 + sl
                dma("sp", xsf[sl], x_d[b, j * 128:(j + 1) * 128, :], writes=[("xsf", sl)])
                blks.append((xsf[sl], ("xsf", sl), j * 128, xsb[sl], ("xsb", sl), sl))
            norm_to_T_multi(blks, hT, "hT", gs1T, modT[:, 0:8, :], b)
        for j in range(NB):
            pb = 2 + j % 2
            for dk in range(8):
                mm(PS[pb].rearrange("p (a b) -> p a b", a=4), hT[:, dk, j * 128:(j + 1) * 128],
                   wu_sb[:, :, dk, :], dk == 0, dk == 7, ["hT", "wu_sb"], [PK[pb]])
            if j % 2 == 0:
                act(u_tm[:, j, :], PS[pb], AF.Copy, [], ["u_tm", PK[pb]])
            else:
                cp("dve", u_tm[:, j, :], PS[pb], [], ["u_tm", PK[pb]])
            for dk in range(8):
                mm(PS[4][:, j * 16:(j + 1) * 16], hT[:, dk, j * 128:(j + 1) * 128], wf_sb[:, dk, :],
                   dk == 0, dk == 7, ["hT", "wf_sb"], [PK[4]])
        tt("dve", Lf, PS[4][:, 0:NB * 16].rearrange("p (a b) -> p a b", a=NB),
           bf_bc.unsqueeze(1).to_broadcast([128, NB, 16]), ALU.add, ["bf_bc"], ["Lf", PK[4]])
        act(Lf, Lf, AF.Exp, ["Lf"], ["Lf"], scale=-1.0)
        act(Lf, Lf, AF.Ln, ["Lf"], ["Lf"], bias=1.0)
        for j in range(NB):
            pb = 2 + j % 2
            for g in range(4):
                o = PS[pb][:, g * 128:(g + 1) * 128]
                mm(o, u_tm[:, j, g * 128:(g + 1) * 128], band_bf[:, g, 0 if j > 0 else 2, :], True, j == 0,
                   ["u_tm", "band_bf"], [PK[pb]])
                if j > 0:
                    mm(o, u_tm[:, j - 1, g * 128:(g + 1) * 128], band_bf[:, g, 1, :], False, True,
                       ["u_tm", "band_bf"], [PK[pb]])
            src = PS[pb].rearrange("p (a b) -> p a b", a=4)
            if j % 2 == 0:
                act(pooledT[:, :, j * 128:(j + 1) * 128], src, AF.Copy, [], ["pooledT", PK[pb]])
            else:
                cp("dve", pooledT[:, :, j * 128:(j + 1) * 128], src, [], ["pooledT", PK[pb]])
        Lflat = Lf.rearrange("p a b -> p (a b)")
        for j in range(NB):
            mm(PS[5][:, j * 16:(j + 1) * 16], tri_f, Lf[:, j, :], True, True, ["tri_f", "Lf"], [PK[5]])
        mm(PS[6][:, 0:NB * 16], ones_f, Lflat, True, True, ["ones_f", "Lf"], [PK[6]])
        cp("dve", Tot.rearrange("p a b -> p (a b)"), PS[6][:, 0:NB * 16], [], ["Tot", PK[6]])
        memset("dve", carry[:, 0, :], 0.0, ["carry"])
        for j in range(1, NB):
            tt("dve", carry[:, j, :], carry[:, j - 1, :], Tot[:, j - 1, :], ALU.add, ["carry", "Tot"], ["carry"])
        tt("dve", Csb.rearrange("p a b -> p (a b)"), PS[5][:, 0:NB * 16], carry.rearrange("p a b -> p (a b)"),
           ALU.add, ["carry"], ["Csb", PK[5]])
        ts("dve", negCf, Csb, -1.0, None, ALU.mult, None, ["Csb"], ["negCf"])
        cp("dve", Chl[:, :, 0:16], negCf, ["negCf"], ["Chl"])
        cp("dve", hif, Chl[:, :, 0:16], ["Chl"], ["hif"])
        tt("dve", Chl[:, :, 16:32], negCf, hif, ALU.subtract, ["negCf", "hif"], ["Chl"])
        for j in range(NB):
            pb = 6 + (j // 8)
            tr(PSB[pb][0:32, (j % 8) * 128:(j % 8 + 1) * 128], Chl[:, j, :], ident_bf, ["Chl", "ident_bf"], [PK[pb]])
        for jb in range((NB + 7) // 8):
            n = min(8, NB - jb * 8) * 128
            cp("dve", negC[:, jb * 1024:jb * 1024 + n], PSB[6 + jb][0:32, 0:n], [], ["negC", PK[6 + jb]])
        if debug and b == 0:
            for nm_, ap_, key_, shp_, dt_ in (("hT", hT, "hT", [128, 8, S], BF16),
                                              ("pooledT", pooledT, "pooledT", [128, 4, S], BF16),
                                              ("Csb", Csb, "Csb", [128, NB, 16], F32)):
                dd = dt("dbg_" + nm_, list(shp_), dt_, kind="ExternalOutput").ap()
                dma("sp", dd, ap_, reads=[key_])
        top[0] = a_top
        P.barrier_all()
        qA = [alloc([128, S], BF16) for _ in range(2)]
        kA = [alloc([128, S], BF16) for _ in range(2)]
        for e_ in range(2):
            memset("dve", kA[e_][64:66, :], 1.0, [("kA", e_)])
        sga = alloc([128, S], BF16)
        sgb = alloc([128, S], BF16)
        vz = alloc([128, NB, 2, 128], BF16)
        PT = [alloc([128, PIECE], BF16) for _ in range(3)]
        rden = alloc([128, PIECE], F32)
        ybn = alloc([128, PIECE], F32)
        memset("pool", vz.rearrange("p a b c -> p (a b c)"), 1.0, ["vz"])
        st_rr = [0]
        for p in range(8):
            wp = wpair[p % 2]
            wpk = ("wpair", p % 2)
            if p + 1 < 8:
                dma("sp", wpair[(p + 1) % 2].rearrange("p a b c -> p (a b c)"), wpairs_b[p + 1],
                    reads=[("wpairs_b", p + 1)], writes=[("wpair", (p + 1) % 2)])
            for e_ in range(2):
                h_ = 2 * p + e_
                dma("sp", qA[e_][64:65, :], negC[h_:h_ + 1, :], reads=["negC"], writes=[("qA", e_)])
                dma("sp", qA[e_][65:66, :], negC[16 + h_:17 + h_, :], reads=["negC"], writes=[("qA", e_)])
            if b == 0:
                for g in range(4 * p, 4 * p + 4):
                    dma("pool", UT_b[g * 4:(g + 1) * 4], UT_d[g * 4:(g + 1) * 4], reads=[("qA", 0)],
                        writes=[("UT_b", g)])
                    dma("pool", V_b[g * 4:(g + 1) * 4], V_d[g * 4:(g + 1) * 4], reads=[("qA", 0)],
                        writes=[("V_b", g)])
            prr = 0
            for wi, which in ((0, "q"), (1, "k"), (3, "ga"), (4, "gb")):
                for tc in range(NTC):
                    pb = 6 + prr % 2
                    prr += 1
                    for dk in range(8):
                        mm(PS[pb][:, 0:TCH], wp[:, wi, dk, :], hT[:, dk, tc * TCH:(tc + 1) * TCH],
                           dk == 0, dk == 7, [wpk, "hT"], [PK[pb]])
                    sl_ = slice(tc * TCH, (tc + 1) * TCH)
                    if which == "q":
                        act(qA[0][0:64, sl_], PS[pb][0:64, 0:TCH], AF.Copy, [], [("qA", 0), PK[pb]], scale=0.125)
                        act(qA[1][0:64, sl_], PS[pb][64:128, 0:TCH], AF.Copy, [], [("qA", 1), PK[pb]], scale=0.125)
                    elif which == "k":
                        cp("dve", kA[0][0:64, sl_], PS[pb][0:64, 0:TCH], [], [("kA", 0), PK[pb]])
                        cp("dve", kA[1][0:64, sl_], PS[pb][64:128, 0:TCH], [], [("kA", 1), PK[pb]])
                    elif which == "ga":
                        act(sga[:, sl_], PS[pb][:, 0:TCH], AF.Sigmoid, [], ["sga", PK[pb]])
                    else:
                        act(sgb[:, sl_], PS[pb][:, 0:TCH], AF.Sigmoid, [], ["sgb", PK[pb]])
            for j0 in range(0, NB, 4):
                nj = min(4, NB - j0)
                pb = 6 + (j0 // 4) % 2
                for jj in range(nj):
                    j = j0 + jj
                    for dk in range(8):
                        mm(PS[pb][:, jj * 128:(jj + 1) * 128], hT[:, dk, j * 128:(j + 1) * 128], wp[:, 2, dk, :],
                           dk == 0, dk == 7, [wpk, "hT"], [PK[pb]])
                src = PS[pb][:, 0:nj * 128].rearrange("p (a b) -> p a b", a=nj)
                act(vz[:, j0:j0 + nj, 0, 0:64], src[:, :, 0:64], AF.Copy, [], ["vz", PK[pb]])
                cp("dve", vz[:, j0:j0 + nj, 1, 64:128], src[:, :, 64:128], [], ["vz", PK[pb]])
            g = p // 2
            half = p % 2
            for tc in range(NTC):
                pb = 6 + tc % 2
                sl_ = slice(tc * TCH, (tc + 1) * TCH)
                mm(PS[pb][:, 0:TCH], wpool_sb[:, g, half * 128:(half + 1) * 128], pooledT[:, g, sl_], True, True,
                   ["wpool_sb", "pooledT"], [PK[pb]])
                P.op("dve", "scalar_tensor_tensor", out=sga[:, sl_], in0=PS[pb][:, 0:TCH], scalar=vecT[:, 2, p:p + 1],
                     in1=sga[:, sl_], op0=ALU.mult, op1=ALU.mult, reads=["vecT", "sga"], writes=["sga", PK[pb]])
            for qh in range(NQH):
                jmax = (qh + 1) * QH // 128
                items = []
                for e in range(2):
                    for j in range(jmax):
                        for pc in range(NPC):
                            ps0 = qh * QH + pc * PIECE
                            ps1 = ps0 + PIECE
                            if ps1 <= j * 128:
                                continue
                            q0 = max(ps0, j * 128)
                            items.append(dict(e=e, j=j, pc=pc, q0=q0, ps1=ps1, n=ps1 - q0, off=q0 - ps0,
                                              diag=(q0 == j * 128), first=(j == 0),
                                              last=(j == min(jmax, ps1 // 128) - 1)))

                def s_stage(it, i):
                    sb_ = 4 + i % 2
                    e = it["e"]
                    j, n, q0, ps1 = it["j"], it["n"], it["q0"], it["ps1"]
                    mm(PS[sb_][:, 0:n], kA[e][0:66, j * 128:(j + 1) * 128], qA[e][0:66, q0:ps1], True, not it["diag"],
                       [("kA", e), ("qA", e)], [PK[sb_]])
                    if it["diag"]:
                        mm(PS[sb_][:, 0:128], ident_bf, neg_bf, False, True, ["ident_bf", "neg_bf"], [PK[sb_]])

                def e_stage(it, i):
                    sb_ = 4 + i % 2
                    pti = i % 3
                    h = 2 * p + it["e"]
                    n = it["n"]
                    act(PT[pti][:, 0:n], PS[sb_][:, 0:n], AF.Exp, ["Csb"], [("PT", pti), PK[sb_]],
                        bias=Csb[:, it["j"], h:h + 1])

                def v_stage(it, i):
                    pti = i % 3
                    n, off, e, j = it["n"], it["off"], it["e"], it["j"]
                    ob = e * NPC + it["pc"]
                    mm(PS[ob][:, off:off + n], vz[:, j, e, :], PT[pti][:, 0:n], it["first"], it["last"],
                       ["vz", ("PT", pti)], [PK[ob]])

                s_stage(items[0], 0)
                for i, it in enumerate(items):
                    if i + 1 < len(items):
                        s_stage(items[i + 1], i + 1)
                    e_stage(it, i)
                    v_stage(it, i)
                for pc in range(NPC):
                    ps0 = qh * QH + pc * PIECE
                    sl_ = slice(ps0, ps0 + PIECE)
                    a0, a1 = PS[pc], PS[NPC + pc]
                    P.op("dve", "reciprocal", out=rden[0:64, :], in_=a0[64:128, 0:PIECE], reads=[],
                         writes=["rden", PK[pc]])
                    P.op("dve", "reciprocal", out=rden[64:128, :], in_=a1[0:64, 0:PIECE], reads=[],
                         writes=["rden", PK[NPC + pc]])
                    tt("dve", ybn[0:64, :], a0[0:64, 0:PIECE], rden[0:64, :], ALU.mult, ["rden"], ["ybn", PK[pc]])
                    tt("dve", ybn[64:128, :], a1[64:128, 0:PIECE], rden[64:128, :], ALU.mult, ["rden"],
                       ["ybn", PK[NPC + pc]])
                    tt("dve", ybn, ybn, sgb[:, sl_], ALU.mult, ["ybn", "sgb"], ["ybn"])
                    tt("dve", yT[:, p, sl_], ybn, sga[:, sl_], ALU.add, ["ybn", "sga"], ["yT"])
        if debug and b == 0:
            dd = dt("dbg_yT", [128, 8, S], BF16, kind="ExternalOutput").ap()
            dma("sp", dd, yT, reads=["yT"])

        top[0] = persist_top
        P.barrier_all()
        GT = alloc([128, 256, 128], BF16)
        _sv = top[0]
        top[0] = ARENA - 16 * 1024 - 64
        wout_sb = alloc([128, 8, 1024], BF16)
        top[0] = _sv
        h2T = alloc([128, 8, 256], BF16)
        xn = alloc([128, 2, 1024], F32)
        gbf = [alloc([128, 16, 8, 16], BF16) for _ in range(2)]
        idxf = [alloc([128, 2, 128], BF16) for _ in range(2)]
        b_top = top[0]
        NRS = 4
        ringU = [alloc([128, 2, 8, 128], BF16) for _ in range(NRS)]
        ringV = [alloc([128, 2, 1024], BF16) for _ in range(NRS)]
        ag = [alloc([128, 256], BF16) for _ in range(3)]
        apb = [alloc([128, 256], BF16) for _ in range(3)]
        tmpo = alloc([128, 512], F32)
        tmpo_j = tmpo.bitcast(BF16)
        xs2 = [alloc([128, 1024], BF16) for _ in range(2)]
        tmpf = [alloc([128, 512], F32) for _ in range(2)]
        b1junk = alloc([128, 1024], BF16)
        assert top[0] <= ARENA - 16 * 1024 - 64, top[0]
        for ti in range(NT):
            tok0 = ti * 256
            if ti == 0:
                dma("sp", wout_sb.rearrange("p a b -> p (a b)"), wout_b, reads=["wout_b"], writes=["wout_sb"])
            for blk in range(2):
                t0 = tok0 + blk * 128
                dma("sp", xn[:, blk, :], x_d[b, t0:t0 + 128, :], writes=[("xn", blk)])
            for blk in range(2):
                t0 = tok0 + blk * 128
                for hf in range(2):
                    pb = blk * 2 + hf
                    for k in range(8):
                        mm(PS[pb], yT[:, k, t0:t0 + 128], wout_sb[:, k, hf * 512:(hf + 1) * 512], k == 0, k == 7,
                           ["yT", "wout_sb"], [PK[pb]])
            for blk in range(2):
                for hf in range(2):
                    pb = blk * 2 + hf
                    tk_ = ("tmpf", hf)
                    tt("dve", tmpf[hf], PS[pb], gt1_bc[:, hf * 512:(hf + 1) * 512], ALU.mult, ["gt1_bc"],
                       [tk_, PK[pb]])
                    tt("pool", xn[:, blk, hf * 512:(hf + 1) * 512], xn[:, blk, hf * 512:(hf + 1) * 512], tmpf[hf],
                       ALU.add, [("xn", blk), tk_], [("xn", blk)])
            norm_to_T_multi([(xn[:, blk, :], ("xn", blk), blk * 128, xs2[blk], ("xs2", blk), 4 + blk)
                             for blk in range(2)], h2T, "h2T", gs2T, modT[:, 24:32, :], b, junk=b1junk, jkey="b1junk")
            if debug and b == 0 and ti == 0:
                dd = dt("dbg_xn", [128, 2, 1024], F32, kind="ExternalOutput").ap()
                dma("sp", dd, xn, reads=[("xn", 0), ("xn", 1)])
                dd = dt("dbg_h2T", [128, 8, 256], BF16, kind="ExternalOutput").ap()
                dma("sp", dd, h2T, reads=["h2T"])
            top[0] = b_top
            P.barrier_all()
            wq_sb = [alloc([128, 4, 8, 128], BF16) for _ in range(2)]
            qpT = alloc([128, 16, 256], BF16)
            sc = alloc([128, 2048], F32)
            wk = alloc([128, 128], F32)
            vl = alloc([128, 16, 16], F32)
            il = alloc([128, 16, 16], U32)
            cand = alloc([128, 8, 256], F32)
            ex = alloc([128, 8, 256], F32)
            cwk = alloc([128, 256], F32)
            cm = alloc([128, 8, 16], F32)
            negm = alloc([128, 8], F32)
            Z = alloc([128, 8], F32)
            for i in range(4):
                dma("sp", wq_sb[i % 2].rearrange("p a b c -> p (a b c)"), wq_b[i], reads=[("wq_b", i)],
                    writes=[("wq_sb", i % 2)])
                for hh in range(4):
                    hp = i * 4 + hh
                    pb = hp % 2
                    for dk in range(8):
                        mm(PS[pb][:, 0:256], wq_sb[i % 2][:, hh, dk, :], h2T[:, dk, :], dk == 0, dk == 7,
                           [("wq_sb", i % 2), "h2T"], [PK[pb]])
                    act(qpT[:, hp, :], PS[pb][:, 0:256], AF.Copy, [], ["qpT", PK[pb]])
            for blk in range(2):
                for hp in range(16):
                    pb = 2 + hp // 4
                    mm(PS[pb][:, (hp % 4) * 128:(hp % 4 + 1) * 128], qpT[:, hp, blk * 128:(blk + 1) * 128],
                       keysT_sb[:, hp, :], True, True, ["qpT", "keysT_sb"], [PK[pb]])
                for q4 in range(4):
                    kk = [("sc", hp_) for hp_ in range(q4 * 4, q4 * 4 + 4)]
                    act(sc[:, q4 * 512:(q4 + 1) * 512], PS[2 + q4], AF.Copy, [], kk + [PK[2 + q4]])
                scs = [sc[:, hp * 128:(hp + 1) * 128] for hp in range(16)]
                for hp in range(16):
                    P.op("dve", "max", out=vl[:, hp, 0:8], in_=scs[hp], reads=[("sc", hp)], writes=[("vl", hp)])
                for hp in range(16):
                    P.op("dve", "max_index", out=il[:, hp, 0:8], in_max=vl[:, hp, 0:8], in_values=scs[hp],
                         reads=[("sc", hp), ("vl", hp)], writes=[("il", hp)])
                for hp in range(16):
                    P.op("dve", "match_replace", out=scs[hp], in_to_replace=vl[:, hp, 0:8], in_values=scs[hp],
                         imm_value=-1e30, reads=[("vl", hp)], writes=[("sc", hp)])
                for hp in range(16):
                    P.op("dve", "max", out=vl[:, hp, 8:16], in_=scs[hp], reads=[("sc", hp)], writes=[("vl", hp)])
                for hp in range(16):
                    P.op("dve", "max_index", out=il[:, hp, 8:16], in_max=vl[:, hp, 8:16], in_values=scs[hp],
                         reads=[("sc", hp), ("vl", hp)], writes=[("il", hp)])
                vl4 = vl.rearrange("q (h p) a -> q h p a", p=2)
                cand4 = cand.rearrange("q h (a b) -> q h a b", a=16)
                tt("dve", cand4, vl4[:, :, 0, :].unsqueeze(3).to_broadcast([128, 8, 16, 16]),
                   vl4[:, :, 1, :].unsqueeze(2).to_broadcast([128, 8, 16, 16]), ALU.add,
                   [("vl", i_) for i_ in range(16)], ["cand"])
                for h in range(8):
                    P.op("dve", "max", out=cm[:, h, 0:8], in_=cand[:, h, :], reads=["cand"], writes=[("cm", h)])
                for h in range(8):
                    P.op("dve", "match_replace", out=ex[:, h, :], in_to_replace=cm[:, h, 0:8], in_values=cand[:, h, :],
                         imm_value=-1e30, reads=["cand", ("cm", h)], writes=[("ex", h)])
                for h in range(8):
                    P.op("dve", "max", out=cm[:, h, 8:16], in_=ex[:, h, :], reads=[("ex", h)], writes=[("cm", h)])
                ts("dve", negm, cm[:, :, 0], -1.0, None, ALU.mult, None, [("cm", i_) for i_ in range(8)], ["negm"])
                for h in range(8):
                    act(ex[:, h, :], cand[:, h, :], AF.Exp, ["cand", "negm"], [("ex", h)], bias=negm[:, h:h + 1])
                for h in range(8):
                    P.op("dve", "scalar_tensor_tensor", out=ex[:, h, :], in0=cand[:, h, :], scalar=cm[:, h, 15:16],
                         in1=ex[:, h, :], op0=ALU.is_ge, op1=ALU.mult, reads=["cand", ("cm", h), ("ex", h)],
                         writes=[("ex", h)])
                exk = [("ex", i_) for i_ in range(8)]
                P.op("dve", "tensor_reduce", out=Z, in_=ex, axis=AX.X, op=ALU.add, reads=exk, writes=["Z"])
                P.op("dve", "reciprocal", out=Z, in_=Z, reads=["Z"], writes=["Z"])
                tt("dve", gbf[blk].rearrange("q a h b -> q h a b"), ex.rearrange("q h (a b) -> q h a b", a=16),
                   Z.unsqueeze(2).unsqueeze(3).to_broadcast([128, 8, 16, 16]), ALU.mult, exk + ["Z"], [("gbf", blk)])
                il4 = il.rearrange("q (h p) a -> q h p a", p=2)
                for pp in range(2):
                    cp("dve", idxf[blk][:, pp, :].rearrange("q (h a) -> q h a", h=8), il4[:, :, pp, :],
                       [("il", i_) for i_ in range(16)], [("idxf", blk)])
            top[0] = b_top
            P.barrier_all()
            idxT = alloc([128, 2, 128], BF16)
            gbdf = alloc([128, 8, 16, 128], BF16)
            hmask_f = alloc([128, 8], F32)
            cp("dve", hmask_f, hmask_bf, ["hmask_bf"], ["hmask_f"])
            NSL = 3
            P1 = [alloc([128, 16, 128], BF16) for _ in range(NSL)]
            P2 = [alloc([128, 16, 128], BF16) for _ in range(NSL)]
            Msb = [alloc([128, 4, 128], BF16) for _ in range(3)]
            MB = (3, 4, 7)
            for blk in range(2):
                for pp in range(2):
                    tr(PSB[0][:, pp * 128:(pp + 1) * 128], idxf[blk][:, pp, :], ident_bf,
                       [("idxf", blk), "ident_bf"], [PK[0]])
                cp("dve", idxT.rearrange("p a b -> p (a b)"), PSB[0][:, 0:256], [], ["idxT", PK[0]])
                for a in range(16):
                    pb = 1 + a // 8
                    tr(PSB[pb][:, (a % 8) * 128:(a % 8 + 1) * 128], gbf[blk][:, a, :, :].rearrange("q h b -> q (h b)"),
                       ident_bf, [("gbf", blk), "ident_bf"], [PK[pb]])
                for hh in range(8):
                    for half in range(2):
                        act(gbdf[:, hh, half * 8:(half + 1) * 8, :].rearrange("p a t -> p (a t)"), PSB[1 + half],
                            AF.Copy, ["hmask_f"], ["gbdf", PK[1 + half]], scale=hmask_f[:, hh:hh + 1])

                def build(sub):
                    sl = sub % NSL
                    t16 = sub * 16
                    tt("dve", P1[sl], iota_bf.unsqueeze(1).to_broadcast([128, 16, 128]),
                       idxT[:, 0, t16:t16 + 16].unsqueeze(2).to_broadcast([128, 16, 128]), ALU.is_equal,
                       ["iota_bf", "idxT"], [("P1", sl)])
                    tt("dve", P2[sl], iota_bf.unsqueeze(1).to_broadcast([128, 16, 128]),
                       idxT[:, 1, t16:t16 + 16].unsqueeze(2).to_broadcast([128, 16, 128]), ALU.is_equal,
                       ["iota_bf", "idxT"], [("P2", sl)])

                def m_stage(q):
                    sl = (q // 4) % NSL
                    pbm = MB[q % 3]
                    for tq in range(4):
                        t = (q % 4) * 4 + tq
                        mm(PS[pbm][:, tq * 128:(tq + 1) * 128], gbdf[:, :, :, (q // 4) * 16 + t], P2[sl][:, t, :], True, True,
                           ["gbdf", ("P2", sl)], [PK[pbm]])

                def me_stage(q):
                    pbm = MB[q % 3]
                    ms = q % 3
                    act(Msb[ms].rearrange("p a b -> p (a b)"), PS[pbm], AF.Copy, [], [("Msb", ms), PK[pbm]])

                def g_stage(q):
                    sl = (q // 4) % NSL
                    ms = q % 3
                    pbg = 5 + q % 2
                    for tq in range(4):
                        t = (q % 4) * 4 + tq
                        mm(PS[pbg][:, tq * 128:(tq + 1) * 128], P1[sl][:, t, :], Msb[ms][:, tq, :], True, True,
                           [("P1", sl), ("Msb", ms)], [PK[pbg]])

                def ge_stage(q):
                    pbg = 5 + q % 2
                    tk = blk * 128 + q * 4
                    dst = GT[:, tk:tk + 4, :].rearrange("p t c -> p (t c)")
                    if q % 4 == 3:
                        cp("dve", dst, PS[pbg], [], ["GT", PK[pbg]])
                    else:
                        act(dst, PS[pbg], AF.Copy, [], ["GT", PK[pbg]])

                NQ = 32
                build(0)
                build(1)
                m_stage(0)
                m_stage(1)
                me_stage(0)
                for q in range(NQ):
                    if q % 4 == 0 and q // 4 + 2 < 8:
                        build(q // 4 + 2)
                    if q + 2 < NQ:
                        m_stage(q + 2)
                    if q + 1 < NQ:
                        me_stage(q + 1)
                    g_stage(q)
                    ge_stage(q)
            if debug and b == 0 and ti == 0:
                dd = dt("dbg_GT", [128, 256, 128], BF16, kind="ExternalOutput").ap()
                dma("sp", dd, GT, reads=["GT"])
            top[0] = b_top
            P.barrier_all()

            def load_group(g):
                sl = g % NRS
                dma("sp", ringU[sl].rearrange("p c k e -> p c (k e)"),
                    UT_b[g * 2:(g + 1) * 2].rearrange("c p f -> p c f"),
                    reads=[("UT_b", g // 2)], writes=[("ringU", sl)])
                dma("sp", ringV[sl], V_b[g * 2:(g + 1) * 2].rearrange("c p f -> p c f"),
                    reads=[("V_b", g // 2)], writes=[("ringV", sl)])

            def mm1(c):
                sl = (c // 2) % NRS
                cc = c % 2
                pb = 4 + c % 4
                for dk in range(8):
                    mm(PS[pb][:, 0:256], ringU[sl][:, cc, dk, :], h2T[:, dk, :], dk == 0, dk == 7,
                       [("ringU", sl), "h2T"], [PK[pb]])

            def mid(c):
                pb = 4 + c % 4
                i = c % 3
                act(ag[i], PS[pb][:, 0:256], AF.Gelu, [], [("ag", i), PK[pb]])
                tt("dve", apb[i], ag[i], GT[:, :, c], ALU.mult, [("ag", i), "GT"], [("apb", i)])

            def mm2(c):
                sl = (c // 2) % NRS
                cc = c % 2
                i = c % 3
                for th in range(2):
                    for dh in range(2):
                        pb = th * 2 + dh
                        mm(PS[pb], apb[i][:, th * 128:(th + 1) * 128], ringV[sl][:, cc, dh * 512:(dh + 1) * 512],
                           c == 0, c == 127, [("apb", i), ("ringV", sl)], [PK[pb]])

            for g_ in range(NRS):
                load_group(g_)
            if ti + 1 < NT:
                dma("sp", wout_sb.rearrange("p a b -> p (a b)"), wout_b, reads=["wout_b"], writes=["wout_sb"])
            mm1(0)
            mm1(1)
            for c in range(128):
                if c + 2 < 128:
                    mm1(c + 2)
                mid(c)
                mm2(c)
                if c % 2 == 1 and c // 2 + NRS < 64:
                    load_group(c // 2 + NRS)
            for th in range(2):
                for dh in range(2):
                    pb = th * 2 + dh
                    tt("dve", tmpo, PS[pb], gt2_bc[:, dh * 512:(dh + 1) * 512], ALU.mult, ["gt2_bc"],
                       ["tmpo", PK[pb]])
                    tt("dve", xn[:, th, dh * 512:(dh + 1) * 512], xn[:, th, dh * 512:(dh + 1) * 512], tmpo,
                       ALU.add, [("xn", th), "tmpo"], [("xn", th)])
                r = rstd_of(xn[:, th, :], [("xn", th)], 4, junk=tmpo_j, jkey="tmpo")
                P.op("dve", "scalar_tensor_tensor", out=xn[:, th, :], in0=xn[:, th, :], scalar=r, in1=fg_bc,
                     op0=ALU.mult, op1=ALU.mult, reads=[("xn", th), "small", "fg_bc"], writes=[("xn", th)])
                t0 = tok0 + th * 128
                dma("sp", out_d[b, t0:t0 + 128, :], xn[:, th, :], reads=[("xn", th)])
    P.emit(es)
    es.close()
    return nc


def blk(w):
    n = w.shape[1]
    return np.ascontiguousarray(w.reshape(8, 128, n).transpose(1, 0, 2))


def prep_shared(inp):
    f = np.float32
    w_in = np.asarray(inp["w_in"][0], f)
    sh = {}
    wp = []
    for p in range(8):
        cols = [512 + p * 128, 1536 + p * 128, 2560 + p * 128, 3600 + p * 128, 4624 + p * 128]
        wp.append(np.stack([blk(w_in[:, c:c + 128]) for c in cols], axis=1))
    sh["wpairs"] = np.ascontiguousarray(np.stack(wp, 0).reshape(8, 128, 5 * 1024))
    sh["wu"] = np.ascontiguousarray(
        np.stack([blk(w_in[:, c * 128:(c + 1) * 128]) for c in range(4)], axis=1).reshape(128, 4 * 1024))
    sh["wf"] = np.ascontiguousarray(blk(w_in[:, 3584:3600]).reshape(128, 128))
    sh["wout"] = np.ascontiguousarray(blk(np.asarray(inp["w_out"][0], f)).reshape(128, 8 * 1024))
    wq = np.asarray(inp["peer_w_query"][0], f)
    wqb = np.stack([blk(wq[:, c * 128:(c + 1) * 128]) for c in range(16)], axis=0)
    sh["wq"] = np.ascontiguousarray(wqb.reshape(4, 4, 128, 1024).transpose(0, 2, 1, 3).reshape(4, 128, 4096))
    keys = np.asarray(inp["peer_sub_keys"][0], f)
    sh["keysT"] = np.ascontiguousarray(keys.transpose(3, 0, 1, 2).reshape(128, 2048))
    sh["wpool"] = np.ascontiguousarray(np.asarray(inp["w_pool"][0], f).transpose(1, 0, 2).reshape(128, 1024))
    U = np.asarray(inp["peer_u"][0], f).reshape(128, 128, 8, 128)
    sh["UT"] = np.ascontiguousarray(U.transpose(1, 3, 2, 0).reshape(128, 128, 1024))
    V = np.asarray(inp["peer_v"][0], f).reshape(128, 128, 1024)
    sh["Vp"] = np.ascontiguousarray(V.transpose(1, 0, 2))
    wm = np.asarray(inp["w_mod"][0], f)
    sh["wmod"] = np.ascontiguousarray(
        np.stack([blk(wm[:, q * 1024:(q + 1) * 1024]) for q in range(6)], 0).reshape(6, 128, 8192))
    sh["bmT"] = np.ascontiguousarray(np.asarray(inp["b_mod"][0], f).reshape(48, 128).T)
    vec = np.stack([np.asarray(inp["norm1_g"][0], f).reshape(8, 128).T,
                    np.asarray(inp["norm2_g"][0], f).reshape(8, 128).T,
                    np.asarray(inp["pool_scale"][0], f).reshape(8, 128).T], axis=1)
    sh["vecT"] = np.ascontiguousarray(vec.reshape(128, 24))
    sh["fg"] = np.ascontiguousarray(np.asarray(inp["final_g"], f).reshape(1, 1024))
    sh["bf"] = np.ascontiguousarray(np.asarray(inp["b_f"][0], f).reshape(1, 16))
    sh["consts"] = make_consts()
    return sh


def core_inputs(sh, x, c):
    m = dict(sh)
    m["x"] = np.ascontiguousarray(x, np.float32)
    nseq = c.shape[0]
    m["cT"] = np.ascontiguousarray(np.asarray(c, np.float32).reshape(nseq, 8, 128).transpose(2, 1, 0)
                                   .reshape(128, 8 * nseq))
    return m


_NC_CACHE = {}


def kernel(**inputs):
    x = np.asarray(inputs["x"], np.float32)
    c = np.asarray(inputs["c"], np.float32)
    B, S, _ = x.shape
    nseq = B // NCORES
    sh = prep_shared(inputs)
    key = (S, nseq)
    if key not in _NC_CACHE:
        _NC_CACHE[key] = build_nc(S, nseq)
    nc = _NC_CACHE[key]
    in_maps = [core_inputs(sh, x[i * nseq:(i + 1) * nseq], c[i * nseq:(i + 1) * nseq]) for i in range(NCORES)]
    res = run_bass_kernel_spmd(nc, in_maps, core_ids=list(range(NCORES)))
    out = np.concatenate([np.asarray(r["out"]) for r in res.results], axis=0)
    return out.astype(np.float32)
```

```python
import numpy as np
from contextlib import ExitStack
import concourse.bass as bass
import concourse.mybir as mybir
from concourse.bass_utils import run_bass_kernel_spmd

F32 = mybir.dt.float32
BF16 = mybir.dt.bfloat16
U32 = mybir.dt.uint32
U8 = mybir.dt.uint8
AF = mybir.ActivationFunctionType
ALU = mybir.AluOpType
AX = mybir.AxisListType

D = 1024
NCORES = 8
EPS = 1e-6
ENGS = ("pe", "act", "dve", "pool", "sp")
ISZ = {F32: 4, BF16: 2, U32: 4, U8: 1}


class Instr:
    __slots__ = ("eng", "idx", "fn", "args", "kwargs", "dma", "deps", "signal",
                 "count", "dsem", "dval")

    def __init__(self, eng, idx, fn, args, kwargs, dma):
        self.eng = eng
        self.idx = idx
        self.fn = fn
        self.args = args
        self.kwargs = kwargs
        self.dma = dma
        self.deps = []
        self.signal = False
        self.count = 0
        self.dsem = None
        self.dval = 0


class Prog:
    def __init__(self, nc, n_dma_sems=40, n_sw=12):
        self.nc = nc
        self.streams = {e: [] for e in ENGS}
        self.reg = {}
        self.barrier = {e: {} for e in ENGS}
        self.n_dma_sems = n_dma_sems
        self.n_sw = n_sw
        self.sw_count = 0
        self.hw_count = 0
        self.dma_last = [None] * n_dma_sems
        self.dma_uses = [0] * n_dma_sems

    def op(self, eng, fn, *args, reads=(), writes=(), dma=False, **kwargs):
        st = self.streams[eng]
        ins = Instr(eng, len(st), fn, args, kwargs, dma)
        deps = {}

        def add(p):
            if p.dma:
                deps[("d", id(p))] = p
            else:
                k = ("e", p.eng)
                q = deps.get(k)
                if q is None or q.idx < p.idx:
                    deps[k] = p

        for r in reads:
            s = self.reg.get(r)
            if s is not None and s[0] is not None:
                add(s[0])
        for w in writes:
            s = self.reg.get(w)
            if s is None:
                continue
            wr = s[0]
            if wr is not None and (wr.dma or dma or wr.eng != eng):
                add(wr)
            for e2, rd in s[1].items():
                if rd.dma or dma or rd.eng != eng:
                    add(rd)
            for rd in s[2]:
                add(rd)
        for p in self.barrier[eng].values():
            if p is not ins:
                add(p)
        self.barrier[eng] = {}
        if dma:
            if eng == "pool":
                slot = self.sw_count % self.n_sw
                self.sw_count += 1
            else:
                slot = self.n_sw + self.hw_count % (self.n_dma_sems - self.n_sw)
                self.hw_count += 1
            prev = self.dma_last[slot]
            if prev is not None:
                deps[("d", id(prev))] = prev
            self.dma_uses[slot] += 1
            ins.dsem = slot
            ins.dval = 16 * self.dma_uses[slot]
            self.dma_last[slot] = ins
        final = []
        for p in deps.values():
            if (not p.dma) and (not dma) and p.eng == eng and eng == "pe":
                continue
            p.signal = True
            final.append(p)
        ins.deps = final
        for r in reads:
            s = self.reg.get(r)
            if s is None:
                s = self.reg[r] = [None, {}, []]
            if dma:
                s[2].append(ins)
            else:
                s[1][eng] = ins
        for w in writes:
            self.reg[w] = [ins, {}, []]
        st.append(ins)
        return ins

    def barrier_all(self, include_sw=False):
        last = {}
        for e in ENGS:
            for q in reversed(self.streams[e]):
                if not q.dma and q.fn is not None:
                    last[e] = q
                    break
        dmas = [d for i_, d in enumerate(self.dma_last) if d is not None and (include_sw or i_ >= self.n_sw)]
        for e in ENGS:
            b = {}
            for e2, p in last.items():
                b[("e", e2)] = p
            for d in dmas:
                b[("d", id(d))] = d
            self.barrier[e] = b

    def emit(self, es):
        nc = self.nc
        self.barrier_all(include_sw=True)
        self.op("sp", None)
        for e in ENGS:
            c = 0
            for ins in self.streams[e]:
                if ins.dma:
                    continue
                if ins.signal:
                    c += 1
                    ins.count = c
        esems = {e: es.enter_context(nc.semaphore("s_" + e)) for e in ENGS}
        dsems = [es.enter_context(nc.semaphore("d%d" % i)) for i in range(self.n_dma_sems)]
        block = es.enter_context(nc.Block())
        handles = {"pe": block.tensor, "act": block.scalar, "dve": block.vector,
                   "pool": block.gpsimd, "sp": block.sync}

        def make(e):
            def body(eng):
                waited = {}
                for ins in self.streams[e]:
                    for p in ins.deps:
                        if p.dma:
                            sem, val, key = dsems[p.dsem], p.dval, ("d", p.dsem)
                        else:
                            sem, val, key = esems[p.eng], p.count, ("e", p.eng)
                        if waited.get(key, 0) >= val:
                            continue
                        waited[key] = val
                        eng.wait_ge(sem, val)
                    if ins.fn is None:
                        continue
                    r = getattr(eng, ins.fn)(*ins.args, **ins.kwargs)
                    if ins.dma:
                        r.then_inc(dsems[ins.dsem], 16)
                    elif ins.signal:
                        r.then_inc(esems[e], 1)
            return body

        for e in ENGS:
            if self.streams[e]:
                handles[e](make(e))


NCONST = 128 + 128 + 1536 + 128 + 8 + 256 + 2048 + 128
C_ID, C_TRI, C_BAND, C_IOTA, C_HM, C_EO, C_SEL, C_NEG = 0, 128, 256, 1792, 1920, 1928, 2184, 4232


def make_consts():
    c = np.zeros((128, NCONST), np.float32)
    c[:, C_ID:C_ID + 128] = np.eye(128)
    s = np.arange(128)
    c[:, C_TRI:C_TRI + 128] = (s[:, None] <= s[None, :])
    band = np.zeros((128, 4, 3, 128), np.float32)
    for g, w in enumerate((2, 4, 8, 16)):
        for t in range(128):
            for ss in range(max(0, t - w + 1), t + 1):
                band[ss, g, 0, t] += 1.0 / w
            band[t, g, 0, t] -= 1.0
            for sp in range(max(0, 128 + t - w + 1), 128):
                band[sp, g, 1, t] += 1.0 / w
            cnt = min(t + 1, w)
            for ss in range(max(0, t - w + 1), t + 1):
                band[ss, g, 2, t] += 1.0 / cnt
            band[t, g, 2, t] -= 1.0
    c[:, C_BAND:C_BAND + 1536] = band.reshape(128, 1536)
    c[:, C_IOTA:C_IOTA + 128] = s[None, :]
    c[:, C_HM:C_HM + 8] = (s[:, None] // 16 == np.arange(8)[None, :])
    eo = np.zeros((128, 2, 128), np.float32)
    eo[:, 0, 0:64] = 1.0
    eo[:, 1, 64:128] = 1.0
    c[:, C_EO:C_EO + 256] = eo.reshape(128, 256)
    sel = np.zeros((128, 16, 128), np.float32)
    for r in range(32):
        sel[r, r % 16, :] = 1.0
    c[:, C_SEL:C_SEL + 2048] = sel.reshape(128, 2048)
    c[:, C_NEG:C_NEG + 128] = np.where(s[:, None] > s[None, :], -30000.0, 0.0)
    return c


def build_nc(S, NSEQ, debug=False):
    NB = S // 128
    NT = S // 256
    PIECE = min(512, S)
    QH = min(1024, S)
    NQH = S // QH
    NPC = QH // PIECE
    TCH = min(512, S)
    NTC = S // TCH

    nc = bass.Bass("TRN2", target_bir_lowering=False)
    dt = nc.dram_tensor

    def din(name, shape, dtype=F32):
        return dt(name, list(shape), dtype, kind="ExternalInput").ap()

    def dscr(name, shape, dtype=BF16):
        return dt(name, list(shape), dtype, kind="Internal").ap()

    x_d = din("x", [NSEQ, S, D])
    cT_d = din("cT", [128, 8 * NSEQ])
    wmod_d = din("wmod", [6, 128, 8 * 1024])
    bmT_d = din("bmT", [128, 48])
    vecT_d = din("vecT", [128, 24])
    fg_d = din("fg", [1, 1024])
    bf_d = din("bf", [1, 16])
    wpairs_d = din("wpairs", [8, 128, 5 * 1024])
    wu_d = din("wu", [128, 4 * 1024])
    wf_d = din("wf", [128, 128])
    wout_d = din("wout", [128, 8 * 1024])
    wq_d = din("wq", [4, 128, 4 * 1024])
    keysT_d = din("keysT", [128, 2048])
    wpool_d = din("wpool", [128, 1024])
    UT_d = din("UT", [128, 128, 1024])
    V_d = din("Vp", [128, 128, 1024])
    consts_d = din("consts", [128, NCONST])
    out_d = dt("out", [NSEQ, S, D], F32, kind="ExternalOutput").ap()

    wpairs_b = dscr("wpairs_b", [8, 128, 5 * 1024])
    wu_b = dscr("wu_b", [128, 4 * 1024])
    wf_b = dscr("wf_b", [128, 128])
    wout_b = dscr("wout_b", [128, 8 * 1024])
    wq_b = dscr("wq_b", [4, 128, 4 * 1024])
    keysT_b = dscr("keysT_b", [128, 2048])
    wpool_b = dscr("wpool_b", [128, 1024])
    UT_b = dscr("UT_b", [128, 128, 1024])
    V_b = dscr("V_b", [128, 128, 1024])

    dbg = {}
    es = ExitStack()
    ARENA = 206 * 1024
    arena = es.enter_context(nc.sbuf_tensor("arena", [128, ARENA], U8))
    banks = [es.enter_context(nc.psum_tensor("psb%d" % i, [128, 512], F32)) for i in range(8)]
    PS = [b[:] for b in banks]
    PSB = [b[:].bitcast(BF16) for b in banks]
    PK = ["ps%d" % i for i in range(8)]
    P = Prog(nc)
    top = [0]

    def alloc(shape, dtype, parts=128):
        n = int(np.prod(shape[1:])) * ISZ[dtype]
        off = (top[0] + 63) // 64 * 64
        top[0] = off + n
        assert top[0] <= ARENA, ("arena overflow", top[0])
        v = arena[:, off:off + n].bitcast(dtype)
        if shape[0] < 128:
            v = v[0:shape[0]]
        if len(shape) == 3:
            v = v.rearrange("p (a b) -> p a b", a=shape[1])
        elif len(shape) == 4:
            v = v.rearrange("p (a b c) -> p a b c", a=shape[1], b=shape[2])
        elif len(shape) == 5:
            v = v.rearrange("p (a b c d) -> p a b c d", a=shape[1], b=shape[2], c=shape[3])
        return v

    def dma(eng, out, in_, reads=(), writes=()):
        return P.op(eng, "dma_start", out=out, in_=in_, reads=reads, writes=writes, dma=True)

    def mm(out, lhsT, rhs, start, stop, reads, writes):
        return P.op("pe", "matmul", out, lhsT=lhsT, rhs=rhs, start=start, stop=stop,
                    reads=reads, writes=writes)

    def tr(out, in_, ident, reads, writes):
        return P.op("pe", "transpose", out=out, in_=in_, identity=ident, reads=reads, writes=writes)

    def act(out, in_, func, reads, writes, **kw):
        return P.op("act", "activation", out=out, in_=in_, func=func, reads=reads, writes=writes, **kw)

    def tt(eng, out, in0, in1, op, reads, writes):
        return P.op(eng, "tensor_tensor", out=out, in0=in0, in1=in1, op=op, reads=reads, writes=writes)

    def ts(eng, out, in0, s1, s2, op0, op1, reads, writes):
        if s2 is None:
            return P.op(eng, "tensor_scalar", out=out, in0=in0, scalar1=s1, scalar2=None, op0=op0,
                        reads=reads, writes=writes)
        return P.op(eng, "tensor_scalar", out=out, in0=in0, scalar1=s1, scalar2=s2, op0=op0, op1=op1,
                    reads=reads, writes=writes)

    def cp(eng, out, in_, reads, writes):
        return P.op(eng, "tensor_copy", out=out, in_=in_, reads=reads, writes=writes)

    def memset(eng, ap, val, writes):
        return P.op(eng, "memset", ap, val, writes=writes)

    ident_bf = alloc([128, 128], BF16)
    ident_f = alloc([128, 128], F32)
    ones_f = alloc([128, 128], F32)
    tri_f = alloc([128, 128], F32)
    tri_bf = alloc([128, 128], BF16)
    neg_bf = alloc([128, 128], BF16)
    band_bf = alloc([128, 4, 3, 128], BF16)
    iota_bf = alloc([128, 128], BF16)
    hmask_bf = alloc([128, 8], BF16)
    keysT_sb = alloc([128, 16, 128], BF16)
    wpool_sb = alloc([128, 4, 256], BF16)
    fg_bc = alloc([128, 1024], F32)
    bf_bc = alloc([128, 16], F32)
    modT = alloc([128, 48, NSEQ], F32)
    vecT = alloc([128, 3, 8], F32)
    bmT = alloc([128, 48], F32)
    cT = alloc([128, 8, NSEQ], F32)
    gs1T = alloc([128, 8, NSEQ], F32)
    gs2T = alloc([128, 8, NSEQ], F32)
    gt1_bc = alloc([128, 1024], F32)
    gt2_bc = alloc([128, 1024], F32)
    yT = alloc([128, 8, S], BF16)
    small = alloc([128, 32], F32)
    persist_top = top[0]

    cst = alloc([128, NCONST], F32)
    dma("sp", cst, consts_d, writes=["cst"])
    dma("sp", vecT, vecT_d.rearrange("p (a b) -> p a b", a=3), writes=["vecT"])
    dma("sp", bmT, bmT_d, writes=["bmT"])
    dma("sp", cT, cT_d.rearrange("p (a b) -> p a b", a=8), writes=["cT"])
    dma("sp", fg_bc, fg_d.partition_broadcast(128), writes=["fg_bc"])
    dma("sp", bf_bc, bf_d.partition_broadcast(128), writes=["bf_bc"])
    dma("pool", wu_b, wu_d, writes=["wu_b"])
    dma("pool", wf_b, wf_d, writes=["wf_b"])
    dma("pool", keysT_b, keysT_d, writes=["keysT_b"])
    dma("pool", wpool_b, wpool_d, writes=["wpool_b"])
    for p in range(8):
        dma("pool", wpairs_b[p], wpairs_d[p], writes=[("wpairs_b", p)])
    dma("pool", wout_b, wout_d, writes=["wout_b"])
    for i in range(4):
        dma("pool", wq_b[i], wq_d[i], writes=[("wq_b", i)])

    cp("dve", ident_bf, cst[:, C_ID:C_ID + 128], ["cst"], ["ident_bf"])
    cp("dve", ident_f, cst[:, C_ID:C_ID + 128], ["cst"], ["ident_f"])
    memset("dve", ones_f, 1.0, ["ones_f"])
    cp("dve", tri_f, cst[:, C_TRI:C_TRI + 128], ["cst"], ["tri_f"])
    cp("dve", tri_bf, cst[:, C_TRI:C_TRI + 128], ["cst"], ["tri_bf"])
    cp("dve", neg_bf, cst[:, C_NEG:C_NEG + 128], ["cst"], ["neg_bf"])
    cp("dve", band_bf.rearrange("p a b c -> p (a b c)"), cst[:, C_BAND:C_BAND + 1536], ["cst"], ["band_bf"])
    cp("dve", iota_bf, cst[:, C_IOTA:C_IOTA + 128], ["cst"], ["iota_bf"])
    cp("dve", hmask_bf, cst[:, C_HM:C_HM + 8], ["cst"], ["hmask_bf"])
    dma("sp", keysT_sb.rearrange("p a b -> p (a b)"), keysT_b, reads=["keysT_b"], writes=["keysT_sb"])
    dma("sp", wpool_sb.rearrange("p a b -> p (a b)"), wpool_b, reads=["wpool_b"], writes=["wpool_sb"])

    wm = [alloc([128, 8, 1024], F32), alloc([128, 8, 1024], F32)]
    for part in range(6):
        w = wm[part % 2]
        wk_ = ("wm", part % 2)
        dma("sp", w.rearrange("p a b -> p (a b)"), wmod_d[part], writes=[wk_])
        pb = 0
        for kc in range(8):
            for dk in range(8):
                mm(PS[pb][:, kc * NSEQ:(kc + 1) * NSEQ], w[:, dk, kc * 128:(kc + 1) * 128], cT[:, dk, :],
                   dk == 0, dk == 7, [wk_, "cT"], [PK[pb]])
        tt("dve", modT[:, part * 8:(part + 1) * 8, :],
           PS[pb][:, 0:8 * NSEQ].rearrange("p (a b) -> p a b", a=8),
           bmT[:, part * 8:(part + 1) * 8].unsqueeze(2).to_broadcast([128, 8, NSEQ]), ALU.add,
           ["bmT"], ["modT", PK[pb]])
    for (gs, gi, sc0) in ((gs1T, 0, 8), (gs2T, 1, 32)):
        ts("dve", gs, modT[:, sc0:sc0 + 8, :], 1.0, None, ALU.add, None, ["modT"], ["gs%d" % gi])
        tt("dve", gs, gs, vecT[:, gi, :].unsqueeze(2).to_broadcast([128, 8, NSEQ]), ALU.mult,
           ["gs%d" % gi, "vecT"], ["gs%d" % gi])
    memset("dve", small, 0.0, ["small"])
    top[0] = persist_top
    P.barrier_all()

    def bcast_gate(b, j0, dst, dkey):
        dg = alloc([128, 8, 128], F32)
        for k in range(8):
            ts("dve", dg[:, k, :], ident_f, modT[:, j0 + k, b:b + 1], None, ALU.mult, None,
               ["ident_f", "modT"], ["dg"])
        for k in range(8):
            pb = 6 + k // 4
            mm(PS[pb][:, (k % 4) * 128:(k % 4 + 1) * 128], ones_f, dg[:, k, :], True, True,
               ["ones_f", "dg"], [PK[pb]])
        cp("dve", dst[:, 0:512], PS[6], [], [dkey, PK[6]])
        cp("dve", dst[:, 512:1024], PS[7], [], [dkey, PK[7]])

    def rstd_of(src, reads, col, junk=None, jkey="junk"):
        if junk is None:
            junk = alloc([128, 1024], BF16)
        memset("dve", small[:, col:col + 1], 0.0, ["small"])
        act(junk, src, AF.Square, reads + ["small"], ["small", jkey], accum_out=small[:, col:col + 1])
        act(small[:, col + 2:col + 3], small[:, col:col + 1], AF.Ln, ["small"], ["small"],
            scale=1.0 / D, bias=EPS)
        act(small[:, col + 1:col + 2], small[:, col + 2:col + 3], AF.Exp, ["small"], ["small"], scale=-0.5)
        return small[:, col + 1:col + 2]

    def norm_to_T(src, skey, dstT, dkey, t0, gsT, shT, b, xs_b, xkey, pb):
        m0 = top[0]
        r = rstd_of(src, [skey], 0)
        ts("dve", xs_b, src, r, None, ALU.mult, None, [skey, "small"], [xkey])
        for k in range(8):
            tr(PSB[pb][:, k * 128:(k + 1) * 128], xs_b[:, k * 128:(k + 1) * 128], ident_bf,
               [xkey, "ident_bf"], [PK[pb]])
        for k in range(8):
            o = dstT[:, k, t0:t0 + 128]
            i = PSB[pb][:, k * 128:(k + 1) * 128]
            if k % 2 == 0:
                act(o, i, AF.Identity, ["gsT", "modT"], [dkey, PK[pb]], scale=gsT[:, k, b:b + 1],
                    bias=shT[:, k, b:b + 1])
            else:
                ts("dve", o, i, gsT[:, k, b:b + 1], shT[:, k, b:b + 1], ALU.mult, ALU.add,
                   ["gsT", "modT"], [dkey, PK[pb]])
        top[0] = m0

    def norm_to_T_multi(blks, dstT, dkey, gsT, shT, b, junk=None, jkey="junk"):
        m0 = top[0]
        if junk is None:
            junk = alloc([128, 1024], BF16)
        n = len(blks)
        for i, (src, skey, t0, xs_b, xkey, pb) in enumerate(blks):
            c0 = 8 + 3 * i
            memset("dve", small[:, c0:c0 + 1], 0.0, [("small", i)])
        for i, (src, skey, t0, xs_b, xkey, pb) in enumerate(blks):
            c0 = 8 + 3 * i
            act(junk, src, AF.Square, [skey, ("small", i)], [("small", i), jkey], accum_out=small[:, c0:c0 + 1])
        for i in range(n):
            c0 = 8 + 3 * i
            act(small[:, c0 + 2:c0 + 3], small[:, c0:c0 + 1], AF.Ln, [("small", i)], [("small", i)],
                scale=1.0 / D, bias=EPS)
        for i in range(n):
            c0 = 8 + 3 * i
            act(small[:, c0 + 1:c0 + 2], small[:, c0 + 2:c0 + 3], AF.Exp, [("small", i)], [("small", i)], scale=-0.5)
        for i, (src, skey, t0, xs_b, xkey, pb) in enumerate(blks):
            c0 = 8 + 3 * i
            ts("dve", xs_b, src, small[:, c0 + 1:c0 + 2], None, ALU.mult, None, [skey, ("small", i)], [xkey])
        for i, (src, skey, t0, xs_b, xkey, pb) in enumerate(blks):
            for k in range(8):
                tr(PSB[pb][:, k * 128:(k + 1) * 128], xs_b[:, k * 128:(k + 1) * 128], ident_bf,
                   [xkey, "ident_bf"], [PK[pb]])
        for i, (src, skey, t0, xs_b, xkey, pb) in enumerate(blks):
            for k in range(8):
                o = dstT[:, k, t0:t0 + 128]
                ii = PSB[pb][:, k * 128:(k + 1) * 128]
                if k % 2 == 0:
                    act(o, ii, AF.Identity, ["gsT", "modT"], [dkey, PK[pb]], scale=gsT[:, k, b:b + 1],
                        bias=shT[:, k, b:b + 1])
                else:
                    ts("dve", o, ii, gsT[:, k, b:b + 1], shT[:, k, b:b + 1], ALU.mult, ALU.add,
                       ["gsT", "modT"], [dkey, PK[pb]])
        top[0] = m0

    for b in range(NSEQ):
        top[0] = persist_top
        P.barrier_all()
        bcast_gate(b, 16, gt1_bc, "gt1_bc")
        bcast_gate(b, 40, gt2_bc, "gt2_bc")
        top[0] = persist_top
        P.barrier_all()
        hT = alloc([128, 8, S], BF16)
        pooledT = alloc([128, 4, S], BF16)
        wpair = [alloc([128, 5, 8, 128], BF16), alloc([128, 5, 8, 128], BF16)]
        Lf = alloc([128, NB, 16], F32)
        Tot = alloc([128, NB, 16], F32)
        carry = alloc([128, NB, 16], F32)
        Csb = alloc([128, NB, 16], F32)
        negCf = alloc([128, NB, 16], F32)
        hif = alloc([128, NB, 16], F32)
        Chl = alloc([128, NB, 32], BF16)
        negC = alloc([32, S], BF16)
        a_top = top[0]
        NXS = min(4, NB)
        xsf = [alloc([128, 1024], F32) for _ in range(NXS)]
        xsb = [alloc([128, 1024], BF16) for _ in range(NXS)]
        u_tm = alloc([128, NB, 512], BF16)
        wu_sb = alloc([128, 4, 8, 128], BF16)
        wf_sb = alloc([128, 8, 16], BF16)
        dma("sp", wu_sb.rearrange("p a b c -> p (a b c)"), wu_b, reads=["wu_b"], writes=["wu_sb"])
        dma("sp", wf_sb.rearrange("p a b -> p (a b)"), wf_b, reads=["wf_b"], writes=["wf_sb"])
        dma("sp", wpair[0].rearrange("p a b c -> p (a b c)"), wpairs_b[0], reads=[("wpairs_b", 0)],
            writes=[("wpair", 0)])
        for j0 in range(0, NB, NXS):
            blks = []
            for sl in range(NXS):
                j = j0 + sl
                dma("sp", xsf[sl], x_d[b, j * 128:(j + 1) * 128, :], writes=[("xsf", sl)])
                blks.append((xsf[sl], ("xsf", sl), j * 128, xsb[sl], ("xsb", sl), sl))
            norm_to_T_multi(blks, hT, "hT", gs1T, modT[:, 0:8, :], b)
        for j in range(NB):
            pb = 2 + j % 2
            for dk in range(8):
                mm(PS[pb].rearrange("p (a b) -> p a b", a=4), hT[:, dk, j * 128:(j + 1) * 128],
                   wu_sb[:, :, dk, :], dk == 0, dk == 7, ["hT", "wu_sb"], [PK[pb]])
            if j % 2 == 0:
                act(u_tm[:, j, :], PS[pb], AF.Copy, [], ["u_tm", PK[pb]])
            else:
                cp("dve", u_tm[:, j, :], PS[pb], [], ["u_tm", PK[pb]])
            for dk in range(8):
                mm(PS[4][:, j * 16:(j + 1) * 16], hT[:, dk, j * 128:(j + 1) * 128], wf_sb[:, dk, :],
                   dk == 0, dk == 7, ["hT", "wf_sb"], [PK[4]])
        tt("dve", Lf, PS[4][:, 0:NB * 16].rearrange("p (a b) -> p a b", a=NB),
           bf_bc.unsqueeze(1).to_broadcast([128, NB, 16]), ALU.add, ["bf_bc"], ["Lf", PK[4]])
        act(Lf, Lf, AF.Exp, ["Lf"], ["Lf"], scale=-1.0)
        act(Lf, Lf, AF.Ln, ["Lf"], ["Lf"], bias=1.0)
        for j in range(NB):
            pb = 2 + j % 2
            for g in range(4):
                o = PS[pb][:, g * 128:(g + 1) * 128]
                mm(o, u_tm[:, j, g * 128:(g + 1) * 128], band_bf[:, g, 0 if j > 0 else 2, :], True, j == 0,
                   ["u_tm", "band_bf"], [PK[pb]])
                if j > 0:
                    mm(o, u_tm[:, j - 1, g * 128:(g + 1) * 128], band_bf[:, g, 1, :], False, True,
                       ["u_tm", "band_bf"], [PK[pb]])
            src = PS[pb].rearrange("p (a b) -> p a b", a=4)
            if j % 2 == 0:
                act(pooledT[:, :, j * 128:(j + 1) * 128], src, AF.Copy, [], ["pooledT", PK[pb]])
            else:
                cp("dve", pooledT[:, :, j * 128:(j + 1) * 128], src, [], ["pooledT", PK[pb]])
        Lflat = Lf.rearrange("p a b -> p (a b)")
        for j in range(NB):
            mm(PS[5][:, j * 16:(j + 1) * 16], tri_f, Lf[:, j, :], True, True, ["tri_f", "Lf"], [PK[5]])
        mm(PS[6][:, 0:NB * 16], ones_f, Lflat, True, True, ["ones_f", "Lf"], [PK[6]])
        cp("dve", Tot.rearrange("p a b -> p (a b)"), PS[6][:, 0:NB * 16], [], ["Tot", PK[6]])
        memset("dve", carry[:, 0, :], 0.0, ["carry"])
        for j in range(1, NB):
            tt("dve", carry[:, j, :], carry[:, j - 1, :], Tot[:, j - 1, :], ALU.add, ["carry", "Tot"], ["carry"])
        tt("dve", Csb.rearrange("p a b -> p (a b)"), PS[5][:, 0:NB * 16], carry.rearrange("p a b -> p (a b)"),
           ALU.add, ["carry"], ["Csb", PK[5]])
        ts("dve", negCf, Csb, -1.0, None, ALU.mult, None, ["Csb"], ["negCf"])
        cp("dve", Chl[:, :, 0:16], negCf, ["negCf"], ["Chl"])
        cp("dve", hif, Chl[:, :, 0:16], ["Chl"], ["hif"])
        tt("dve", Chl[:, :, 16:32], negCf, hif, ALU.subtract, ["negCf", "hif"], ["Chl"])
        for j in range(NB):
            pb = 6 + (j // 8)
            tr(PSB[pb][0:32, (j % 8) * 128:(j % 8 + 1) * 128], Chl[:, j, :], ident_bf, ["Chl", "ident_bf"], [PK[pb]])
        for jb in range((NB + 7) // 8):
            n = min(8, NB - jb * 8) * 128
            cp("dve", negC[:, jb * 1024:jb * 1024 + n], PSB[6 + jb][0:32, 0:n], [], ["negC", PK[6 + jb]])
        if debug and b == 0:
            for nm_, ap_, key_, shp_, dt_ in (("hT", hT, "hT", [128, 8, S], BF16),
                                              ("pooledT", pooledT, "pooledT", [128, 4, S], BF16),
                                              ("Csb", Csb, "Csb", [128, NB, 16], F32)):
                dd = dt("dbg_" + nm_, list(shp_), dt_, kind="ExternalOutput").ap()
                dma("sp", dd, ap_, reads=[key_])
        top[0] = a_top
        P.barrier_all()
        qA = [alloc([128, S], BF16) for _ in range(2)]
        kA = [alloc([128, S], BF16) for _ in range(2)]
        for e_ in range(2):
            memset("dve", kA[e_][64:66, :], 1.0, [("kA", e_)])
        sga = alloc([128, S], BF16)
        sgb = alloc([128, S], BF16)
        vz = alloc([128, NB, 2, 128], BF16)
        PT = [alloc([128, PIECE], BF16) for _ in range(3)]
        rden = alloc([128, PIECE], F32)
        ybn = alloc([128, PIECE], F32)
        memset("pool", vz.rearrange("p a b c -> p (a b c)"), 1.0, ["vz"])
        st_rr = [0]
        for p in range(8):
            wp = wpair[p % 2]
            wpk = ("wpair", p % 2)
            if p + 1 < 8:
                dma("sp", wpair[(p + 1) % 2].rearrange("p a b c -> p (a b c)"), wpairs_b[p + 1],
                    reads=[("wpairs_b", p + 1)], writes=[("wpair", (p + 1) % 2)])
            for e_ in range(2):
                h_ = 2 * p + e_
                dma("sp", qA[e_][64:65, :], negC[h_:h_ + 1, :], reads=["negC"], writes=[("qA", e_)])
                dma("sp", qA[e_][65:66, :], negC[16 + h_:17 + h_, :], reads=["negC"], writes=[("qA", e_)])
            if b == 0:
                for g in range(4 * p, 4 * p + 4):
                    dma("pool", UT_b[g * 4:(g + 1) * 4], UT_d[g * 4:(g + 1) * 4], reads=[("qA", 0)],
                        writes=[("UT_b", g)])
                    dma("pool", V_b[g * 4:(g + 1) * 4], V_d[g * 4:(g + 1) * 4], reads=[("qA", 0)],
                        writes=[("V_b", g)])
            prr = 0
            for wi, which in ((0, "q"), (1, "k"), (3, "ga"), (4, "gb")):
                for tc in range(NTC):
                    pb = 6 + prr % 2
                    prr += 1
                    for dk in range(8):
                        mm(PS[pb][:, 0:TCH], wp[:, wi, dk, :], hT[:, dk, tc * TCH:(tc + 1) * TCH],
                           dk == 0, dk == 7, [wpk, "hT"], [PK[pb]])
                    sl_ = slice(tc * TCH, (tc + 1) * TCH)
                    if which == "q":
                        act(qA[0][0:64, sl_], PS[pb][0:64, 0:TCH], AF.Copy, [], [("qA", 0), PK[pb]], scale=0.125)
                        act(qA[1][0:64, sl_], PS[pb][64:128, 0:TCH], AF.Copy, [], [("qA", 1), PK[pb]], scale=0.125)
                    elif which == "k":
                        cp("dve", kA[0][0:64, sl_], PS[pb][0:64, 0:TCH], [], [("kA", 0), PK[pb]])
                        cp("dve", kA[1][0:64, sl_], PS[pb][64:128, 0:TCH], [], [("kA", 1), PK[pb]])
                    elif which == "ga":
                        act(sga[:, sl_], PS[pb][:, 0:TCH], AF.Sigmoid, [], ["sga", PK[pb]])
                    else:
                        act(sgb[:, sl_], PS[pb][:, 0:TCH], AF.Sigmoid, [], ["sgb", PK[pb]])
            for j0 in range(0, NB, 4):
                nj = min(4, NB - j0)
                pb = 6 + (j0 // 4) % 2
                for jj in range(nj):
                    j = j0 + jj
                    for dk in range(8):
                        mm(PS[pb][:, jj * 128:(jj + 1) * 128], hT[:, dk, j * 128:(j + 1) * 128], wp[:, 2, dk, :],
                           dk == 0, dk == 7, [wpk, "hT"], [PK[pb]])
                src = PS[pb][:, 0:nj * 128].rearrange("p (a b) -> p a b", a=nj)
                act(vz[:, j0:j0 + nj, 0, 0:64], src[:, :, 0:64], AF.Copy, [], ["vz", PK[pb]])
                cp("dve", vz[:, j0:j0 + nj, 1, 64:128], src[:, :, 64:128], [], ["vz", PK[pb]])
            g = p // 2
            half = p % 2
            for tc in range(NTC):
                pb = 6 + tc % 2
                sl_ = slice(tc * TCH, (tc + 1) * TCH)
                mm(PS[pb][:, 0:TCH], wpool_sb[:, g, half * 128:(half + 1) * 128], pooledT[:, g, sl_], True, True,
                   ["wpool_sb", "pooledT"], [PK[pb]])
                P.op("dve", "scalar_tensor_tensor", out=sga[:, sl_], in0=PS[pb][:, 0:TCH], scalar=vecT[:, 2, p:p + 1],
                     in1=sga[:, sl_], op0=ALU.mult, op1=ALU.mult, reads=["vecT", "sga"], writes=["sga", PK[pb]])
            for qh in range(NQH):
                jmax = (qh + 1) * QH // 128
                items = []
                for e in range(2):
                    for j in range(jmax):
                        for pc in range(NPC):
                            ps0 = qh * QH + pc * PIECE
                            ps1 = ps0 + PIECE
                            if ps1 <= j * 128:
                                continue
                            q0 = max(ps0, j * 128)
                            items.append(dict(e=e, j=j, pc=pc, q0=q0, ps1=ps1, n=ps1 - q0, off=q0 - ps0,
                                              diag=(q0 == j * 128), first=(j == 0),
                                              last=(j == min(jmax, ps1 // 128) - 1)))

                def s_stage(it, i):
                    sb_ = 4 + i % 2
                    e = it["e"]
                    j, n, q0, ps1 = it["j"], it["n"], it["q0"], it["ps1"]
                    mm(PS[sb_][:, 0:n], kA[e][0:66, j * 128:(j + 1) * 128], qA[e][0:66, q0:ps1], True, not it["diag"],
                       [("kA", e), ("qA", e)], [PK[sb_]])
                    if it["diag"]:
                        mm(PS[sb_][:, 0:128], ident_bf, neg_bf, False, True, ["ident_bf", "neg_bf"], [PK[sb_]])

                def e_stage(it, i):
                    sb_ = 4 + i % 2
                    pti = i % 3
                    h = 2 * p + it["e"]
                    n = it["n"]
                    act(PT[pti][:, 0:n], PS[sb_][:, 0:n], AF.Exp, ["Csb"], [("PT", pti), PK[sb_]],
                        bias=Csb[:, it["j"], h:h + 1])

                def v_stage(it, i):
                    pti = i % 3
                    n, off, e, j = it["n"], it["off"], it["e"], it["j"]
                    ob = e * NPC + it["pc"]
                    mm(PS[ob][:, off:off + n], vz[:, j, e, :], PT[pti][:, 0:n], it["first"], it["last"],
                       ["vz", ("PT", pti)], [PK[ob]])

                s_stage(items[0], 0)
                for i, it in enumerate(items):
                    if i + 1 < len(items):
                        s_stage(items[i + 1], i + 1)
                    e_stage(it, i)
                    v_stage(it, i)
                for pc in range(NPC):
                    ps0 = qh * QH + pc * PIECE
                    sl_ = slice(ps0, ps0 + PIECE)
                    a0, a1 = PS[pc], PS[NPC + pc]
                    P.op("dve", "reciprocal", out=rden[0:64, :], in_=a0[64:128, 0:PIECE], reads=[],
                         writes=["rden", PK[pc]])
                    P.op("dve", "reciprocal", out=rden[64:128, :], in_=a1[0:64, 0:PIECE], reads=[],
                         writes=["rden", PK[NPC + pc]])
                    tt("dve", ybn[0:64, :], a0[0:64, 0:PIECE], rden[0:64, :], ALU.mult, ["rden"], ["ybn", PK[pc]])
                    tt("dve", ybn[64:128, :], a1[64:128, 0:PIECE], rden[64:128, :], ALU.mult, ["rden"],
                       ["ybn", PK[NPC + pc]])
                    tt("dve", ybn, ybn, sgb[:, sl_], ALU.mult, ["ybn", "sgb"], ["ybn"])
                    tt("dve", yT[:, p, sl_], ybn, sga[:, sl_], ALU.add, ["ybn", "sga"], ["yT"])
        if debug and b == 0:
            dd = dt("dbg_yT", [128, 8, S], BF16, kind="ExternalOutput").ap()
            dma("sp", dd, yT, reads=["yT"])

        top[0] = persist_top
        P.barrier_all()
        GT = alloc([128, 256, 128], BF16)
        _sv = top[0]
        top[0] = ARENA - 16 * 1024 - 64
        wout_sb = alloc([128, 8, 1024], BF16)
        top[0] = _sv
        h2T = alloc([128, 8, 256], BF16)
        xn = alloc([128, 2, 1024], F32)
        gbf = [alloc([128, 16, 8, 16], BF16) for _ in range(2)]
        idxf = [alloc([128, 2, 128], BF16) for _ in range(2)]
        b_top = top[0]
        NRS = 4
        ringU = [alloc([128, 2, 8, 128], BF16) for _ in range(NRS)]
        ringV = [alloc([128, 2, 1024], BF16) for _ in range(NRS)]
        ag = [alloc([128, 256], BF16) for _ in range(3)]
        apb = [alloc([128, 256], BF16) for _ in range(3)]
        tmpo = alloc([128, 512], F32)
        tmpo_j = tmpo.bitcast(BF16)
        xs2 = [alloc([128, 1024], BF16) for _ in range(2)]
        tmpf = [alloc([128, 512], F32) for _ in range(2)]
        b1junk = alloc([128, 1024], BF16)
        assert top[0] <= ARENA - 16 * 1024 - 64, top[0]
        for ti in range(NT):
            tok0 = ti * 256
            if ti == 0:
                dma("sp", wout_sb.rearrange("p a b -> p (a b)"), wout_b, reads=["wout_b"], writes=["wout_sb"])
            for blk in range(2):
                t0 = tok0 + blk * 128
                dma("sp", xn[:, blk, :], x_d[b, t0:t0 + 128, :], writes=[("xn", blk)])
            for blk in range(2):
                t0 = tok0 + blk * 128
                for hf in range(2):
                    pb = blk * 2 + hf
                    for k in range(8):
                        mm(PS[pb], yT[:, k, t0:t0 + 128], wout_sb[:, k, hf * 512:(hf + 1) * 512], k == 0, k == 7,
                           ["yT", "wout_sb"], [PK[pb]])
            for blk in range(2):
                for hf in range(2):
                    pb = blk * 2 + hf
                    tk_ = ("tmpf", hf)
                    tt("dve", tmpf[hf], PS[pb], gt1_bc[:, hf * 512:(hf + 1) * 512], ALU.mult, ["gt1_bc"],
                       [tk_, PK[pb]])
                    tt("pool", xn[:, blk, hf * 512:(hf + 1) * 512], xn[:, blk, hf * 512:(hf + 1) * 512], tmpf[hf],
                       ALU.add, [("xn", blk), tk_], [("xn", blk)])
            norm_to_T_multi([(xn[:, blk, :], ("xn", blk), blk * 128, xs2[blk], ("xs2", blk), 4 + blk)
                             for blk in range(2)], h2T, "h2T", gs2T, modT[:, 24:32, :], b, junk=b1junk, jkey="b1junk")
            if debug and b == 0 and ti == 0:
                dd = dt("dbg_xn", [128, 2, 1024], F32, kind="ExternalOutput").ap()
                dma("sp", dd, xn, reads=[("xn", 0), ("xn", 1)])
                dd = dt("dbg_h2T", [128, 8, 256], BF16, kind="ExternalOutput").ap()
                dma("sp", dd, h2T, reads=["h2T"])
            top[0] = b_top
            P.barrier_all()
            wq_sb = [alloc([128, 4, 8, 128], BF16) for _ in range(2)]
            qpT = alloc([128, 16, 256], BF16)
            sc = alloc([128, 2048], F32)
            wk = alloc([128, 128], F32)
            vl = alloc([128, 16, 16], F32)
            il = alloc([128, 16, 16], U32)
            cand = alloc([128, 8, 256], F32)
            ex = alloc([128, 8, 256], F32)
            cwk = alloc([128, 256], F32)
            cm = alloc([128, 8, 16], F32)
            negm = alloc([128, 8], F32)
            Z = alloc([128, 8], F32)
            for i in range(4):
                dma("sp", wq_sb[i % 2].rearrange("p a b c -> p (a b c)"), wq_b[i], reads=[("wq_b", i)],
                    writes=[("wq_sb", i % 2)])
                for hh in range(4):
                    hp = i * 4 + hh
                    pb = hp % 2
                    for dk in range(8):
                        mm(PS[pb][:, 0:256], wq_sb[i % 2][:, hh, dk, :], h2T[:, dk, :], dk == 0, dk == 7,
                           [("wq_sb", i % 2), "h2T"], [PK[pb]])
                    act(qpT[:, hp, :], PS[pb][:, 0:256], AF.Copy, [], ["qpT", PK[pb]])
            for blk in range(2):
                for hp in range(16):
                    pb = 2 + hp // 4
                    mm(PS[pb][:, (hp % 4) * 128:(hp % 4 + 1) * 128], qpT[:, hp, blk * 128:(blk + 1) * 128],
                       keysT_sb[:, hp, :], True, True, ["qpT", "keysT_sb"], [PK[pb]])
                for q4 in range(4):
                    kk = [("sc", hp_) for hp_ in range(q4 * 4, q4 * 4 + 4)]
                    act(sc[:, q4 * 512:(q4 + 1) * 512], PS[2 + q4], AF.Copy, [], kk + [PK[2 + q4]])
                scs = [sc[:, hp * 128:(hp + 1) * 128] for hp in range(16)]
                for hp in range(16):
                    P.op("dve", "max", out=vl[:, hp, 0:8], in_=scs[hp], reads=[("sc", hp)], writes=[("vl", hp)])
                for hp in range(16):
                    P.op("dve", "max_index", out=il[:, hp, 0:8], in_max=vl[:, hp, 0:8], in_values=scs[hp],
                         reads=[("sc", hp), ("vl", hp)], writes=[("il", hp)])
                for hp in range(16):
                    P.op("dve", "match_replace", out=scs[hp], in_to_replace=vl[:, hp, 0:8], in_values=scs[hp],
                         imm_value=-1e30, reads=[("vl", hp)], writes=[("sc", hp)])
                for hp in range(16):
                    P.op("dve", "max", out=vl[:, hp, 8:16], in_=scs[hp], reads=[("sc", hp)], writes=[("vl", hp)])
                for hp in range(16):
                    P.op("dve", "max_index", out=il[:, hp, 8:16], in_max=vl[:, hp, 8:16], in_values=scs[hp],
                         reads=[("sc", hp), ("vl", hp)], writes=[("il", hp)])
                vl4 = vl.rearrange("q (h p) a -> q h p a", p=2)
                cand4 = cand.rearrange("q h (a b) -> q h a b", a=16)
                tt("dve", cand4, vl4[:, :, 0, :].unsqueeze(3).to_broadcast([128, 8, 16, 16]),
                   vl4[:, :, 1, :].unsqueeze(2).to_broadcast([128, 8, 16, 16]), ALU.add,
                   [("vl", i_) for i_ in range(16)], ["cand"])
                for h in range(8):
                    P.op("dve", "max", out=cm[:, h, 0:8], in_=cand[:, h, :], reads=["cand"], writes=[("cm", h)])
                for h in range(8):
                    P.op("dve", "match_replace", out=ex[:, h, :], in_to_replace=cm[:, h, 0:8], in_values=cand[:, h, :],
                         imm_value=-1e30, reads=["cand", ("cm", h)], writes=[("ex", h)])
                for h in range(8):
                    P.op("dve", "max", out=cm[:, h, 8:16], in_=ex[:, h, :], reads=[("ex", h)], writes=[("cm", h)])
                ts("dve", negm, cm[:, :, 0], -1.0, None, ALU.mult, None, [("cm", i_) for i_ in range(8)], ["negm"])
                for h in range(8):
                    act(ex[:, h, :], cand[:, h, :], AF.Exp, ["cand", "negm"], [("ex", h)], bias=negm[:, h:h + 1])
                for h in range(8):
                    P.op("dve", "scalar_tensor_tensor", out=ex[:, h, :], in0=cand[:, h, :], scalar=cm[:, h, 15:16],
                         in1=ex[:, h, :], op0=ALU.is_ge, op1=ALU.mult, reads=["cand", ("cm", h), ("ex", h)],
                         writes=[("ex", h)])
                exk = [("ex", i_) for i_ in range(8)]
                P.op("dve", "tensor_reduce", out=Z, in_=ex, axis=AX.X, op=ALU.add, reads=exk, writes=["Z"])
                P.op("dve", "reciprocal", out=Z, in_=Z, reads=["Z"], writes=["Z"])
                tt("dve", gbf[blk].rearrange("q a h b -> q h a b"), ex.rearrange("q h (a b) -> q h a b", a=16),
                   Z.unsqueeze(2).unsqueeze(3).to_broadcast([128, 8, 16, 16]), ALU.mult, exk + ["Z"], [("gbf", blk)])
                il4 = il.rearrange("q (h p) a -> q h p a", p=2)
                for pp in range(2):
                    cp("dve", idxf[blk][:, pp, :].rearrange("q (h a) -> q h a", h=8), il4[:, :, pp, :],
                       [("il", i_) for i_ in range(16)], [("idxf", blk)])
            top[0] = b_top
            P.barrier_all()
            idxT = alloc([128, 2, 128], BF16)
            gbdf = alloc([128, 8, 16, 128], BF16)
            hmask_f = alloc([128, 8], F32)
            cp("dve", hmask_f, hmask_bf, ["hmask_bf"], ["hmask_f"])
            NSL = 3
            P1 = [alloc([128, 16, 128], BF16) for _ in range(NSL)]
            P2 = [alloc([128, 16, 128], BF16) for _ in range(NSL)]
            Msb = [alloc([128, 4, 128], BF16) for _ in range(3)]
            MB = (3, 4, 7)
            for blk in range(2):
                for pp in range(2):
                    tr(PSB[0][:, pp * 128:(pp + 1) * 128], idxf[blk][:, pp, :], ident_bf,
                       [("idxf", blk), "ident_bf"], [PK[0]])
                cp("dve", idxT.rearrange("p a b -> p (a b)"), PSB[0][:, 0:256], [], ["idxT", PK[0]])
                for a in range(16):
                    pb = 1 + a // 8
                    tr(PSB[pb][:, (a % 8) * 128:(a % 8 + 1) * 128], gbf[blk][:, a, :, :].rearrange("q h b -> q (h b)"),
                       ident_bf, [("gbf", blk), "ident_bf"], [PK[pb]])
                for hh in range(8):
                    for half in range(2):
                        act(gbdf[:, hh, half * 8:(half + 1) * 8, :].rearrange("p a t -> p (a t)"), PSB[1 + half],
                            AF.Copy, ["hmask_f"], ["gbdf", PK[1 + half]], scale=hmask_f[:, hh:hh + 1])

                def build(sub):
                    sl = sub % NSL
                    t16 = sub * 16
                    tt("dve", P1[sl], iota_bf.unsqueeze(1).to_broadcast([128, 16, 128]),
                       idxT[:, 0, t16:t16 + 16].unsqueeze(2).to_broadcast([128, 16, 128]), ALU.is_equal,
                       ["iota_bf", "idxT"], [("P1", sl)])
                    tt("dve", P2[sl], iota_bf.unsqueeze(1).to_broadcast([128, 16, 128]),
                       idxT[:, 1, t16:t16 + 16].unsqueeze(2).to_broadcast([128, 16, 128]), ALU.is_equal,
                       ["iota_bf", "idxT"], [("P2", sl)])

                def m_stage(q):
                    sl = (q // 4) % NSL
                    pbm = MB[q % 3]
                    for tq in range(4):
                        t = (q % 4) * 4 + tq
                        mm(PS[pbm][:, tq * 128:(tq + 1) * 128], gbdf[:, :, :, (q // 4) * 16 + t], P2[sl][:, t, :], True, True,
                           ["gbdf", ("P2", sl)], [PK[pbm]])

                def me_stage(q):
                    pbm = MB[q % 3]
                    ms = q % 3
                    act(Msb[ms].rearrange("p a b -> p (a b)"), PS[pbm], AF.Copy, [], [("Msb", ms), PK[pbm]])

                def g_stage(q):
                    sl = (q // 4) % NSL
                    ms = q % 3
                    pbg = 5 + q % 2
                    for tq in range(4):
                        t = (q % 4) * 4 + tq
                        mm(PS[pbg][:, tq * 128:(tq + 1) * 128], P1[sl][:, t, :], Msb[ms][:, tq, :], True, True,
                           [("P1", sl), ("Msb", ms)], [PK[pbg]])

                def ge_stage(q):
                    pbg = 5 + q % 2
                    tk = blk * 128 + q * 4
                    dst = GT[:, tk:tk + 4, :].rearrange("p t c -> p (t c)")
                    if q % 8 == 7:
                        cp("dve", dst, PS[pbg], [], ["GT", PK[pbg]])
                    else:
                        act(dst, PS[pbg], AF.Copy, [], ["GT", PK[pbg]])

                NQ = 32
                build(0)
                build(1)
                m_stage(0)
                m_stage(1)
                me_stage(0)
                for q in range(NQ):
                    if q % 4 == 0 and q // 4 + 2 < 8:
                        build(q // 4 + 2)
                    if q + 2 < NQ:
                        m_stage(q + 2)
                    if q + 1 < NQ:
                        me_stage(q + 1)
                    g_stage(q)
                    ge_stage(q)
            if debug and b == 0 and ti == 0:
                dd = dt("dbg_GT", [128, 256, 128], BF16, kind="ExternalOutput").ap()
                dma("sp", dd, GT, reads=["GT"])
            top[0] = b_top
            P.barrier_all()

            def load_group(g):
                sl = g % NRS
                dma("sp", ringU[sl].rearrange("p c k e -> p c (k e)"),
                    UT_b[g * 2:(g + 1) * 2].rearrange("c p f -> p c f"),
                    reads=[("UT_b", g // 2)], writes=[("ringU", sl)])
                dma("sp", ringV[sl], V_b[g * 2:(g + 1) * 2].rearrange("c p f -> p c f"),
                    reads=[("V_b", g // 2)], writes=[("ringV", sl)])

            def mm1(c):
                sl = (c // 2) % NRS
                cc = c % 2
                pb = 4 + c % 4
                for dk in range(8):
                    mm(PS[pb][:, 0:256], ringU[sl][:, cc, dk, :], h2T[:, dk, :], dk == 0, dk == 7,
                       [("ringU", sl), "h2T"], [PK[pb]])

            def mid(c):
                pb = 4 + c % 4
                i = c % 3
                act(ag[i], PS[pb][:, 0:256], AF.Gelu, [], [("ag", i), PK[pb]])
                tt("dve", apb[i], ag[i], GT[:, :, c], ALU.mult, [("ag", i), "GT"], [("apb", i)])

            def mm2(c):
                sl = (c // 2) % NRS
                cc = c % 2
                i = c % 3
                for th in range(2):
                    for dh in range(2):
                        pb = th * 2 + dh
                        mm(PS[pb], apb[i][:, th * 128:(th + 1) * 128], ringV[sl][:, cc, dh * 512:(dh + 1) * 512],
                           c == 0, c == 127, [("apb", i), ("ringV", sl)], [PK[pb]])

            for g_ in range(NRS):
                load_group(g_)
            if ti + 1 < NT:
                dma("sp", wout_sb.rearrange("p a b -> p (a b)"), wout_b, reads=["wout_b"], writes=["wout_sb"])
            mm1(0)
            mm1(1)
            for c in range(128):
                if c + 2 < 128:
                    mm1(c + 2)
                mid(c)
                mm2(c)
                if c % 2 == 1 and c // 2 + NRS < 64:
                    load_group(c // 2 + NRS)
            for th in range(2):
                for dh in range(2):
                    pb = th * 2 + dh
                    tt("dve", tmpo, PS[pb], gt2_bc[:, dh * 512:(dh + 1) * 512], ALU.mult, ["gt2_bc"],
                       ["tmpo", PK[pb]])
                    tt("dve", xn[:, th, dh * 512:(dh + 1) * 512], xn[:, th, dh * 512:(dh + 1) * 512], tmpo,
                       ALU.add, [("xn", th), "tmpo"], [("xn", th)])
                r = rstd_of(xn[:, th, :], [("xn", th)], 4, junk=tmpo_j, jkey="tmpo")
                P.op("dve", "scalar_tensor_tensor", out=xn[:, th, :], in0=xn[:, th, :], scalar=r, in1=fg_bc,
                     op0=ALU.mult, op1=ALU.mult, reads=[("xn", th), "small", "fg_bc"], writes=[("xn", th)])
                t0 = tok0 + th * 128
                dma("sp", out_d[b, t0:t0 + 128, :], xn[:, th, :], reads=[("xn", th)])
    P.emit(es)
    es.close()
    return nc


def blk(w):
    n = w.shape[1]
    return np.ascontiguousarray(w.reshape(8, 128, n).transpose(1, 0, 2))


def prep_shared(inp):
    f = np.float32
    w_in = np.asarray(inp["w_in"][0], f)
    sh = {}
    wp = []
    for p in range(8):
        cols = [512 + p * 128, 1536 + p * 128, 2560 + p * 128, 3600 + p * 128, 4624 + p * 128]
        wp.append(np.stack([blk(w_in[:, c:c + 128]) for c in cols], axis=1))
    sh["wpairs"] = np.ascontiguousarray(np.stack(wp, 0).reshape(8, 128, 5 * 1024))
    sh["wu"] = np.ascontiguousarray(
        np.stack([blk(w_in[:, c * 128:(c + 1) * 128]) for c in range(4)], axis=1).reshape(128, 4 * 1024))
    sh["wf"] = np.ascontiguousarray(blk(w_in[:, 3584:3600]).reshape(128, 128))
    sh["wout"] = np.ascontiguousarray(blk(np.asarray(inp["w_out"][0], f)).reshape(128, 8 * 1024))
    wq = np.asarray(inp["peer_w_query"][0], f)
    wqb = np.stack([blk(wq[:, c * 128:(c + 1) * 128]) for c in range(16)], axis=0)
    sh["wq"] = np.ascontiguousarray(wqb.reshape(4, 4, 128, 1024).transpose(0, 2, 1, 3).reshape(4, 128, 4096))
    keys = np.asarray(inp["peer_sub_keys"][0], f)
    sh["keysT"] = np.ascontiguousarray(keys.transpose(3, 0, 1, 2).reshape(128, 2048))
    sh["wpool"] = np.ascontiguousarray(np.asarray(inp["w_pool"][0], f).transpose(1, 0, 2).reshape(128, 1024))
    U = np.asarray(inp["peer_u"][0], f).reshape(128, 128, 8, 128)
    sh["UT"] = np.ascontiguousarray(U.transpose(1, 3, 2, 0).reshape(128, 128, 1024))
    V = np.asarray(inp["peer_v"][0], f).reshape(128, 128, 1024)
    sh["Vp"] = np.ascontiguousarray(V.transpose(1, 0, 2))
    wm = np.asarray(inp["w_mod"][0], f)
    sh["wmod"] = np.ascontiguousarray(
        np.stack([blk(wm[:, q * 1024:(q + 1) * 1024]) for q in range(6)], 0).reshape(6, 128, 8192))
    sh["bmT"] = np.ascontiguousarray(np.asarray(inp["b_mod"][0], f).reshape(48, 128).T)
    vec = np.stack([np.asarray(inp["norm1_g"][0], f).reshape(8, 128).T,
                    np.asarray(inp["norm2_g"][0], f).reshape(8, 128).T,
                    np.asarray(inp["pool_scale"][0], f).reshape(8, 128).T], axis=1)
    sh["vecT"] = np.ascontiguousarray(vec.reshape(128, 24))
    sh["fg"] = np.ascontiguousarray(np.asarray(inp["final_g"], f).reshape(1, 1024))
    sh["bf"] = np.ascontiguousarray(np.asarray(inp["b_f"][0], f).reshape(1, 16))
    sh["consts"] = make_consts()
    return sh


def core_inputs(sh, x, c):
    m = dict(sh)
    m["x"] = np.ascontiguousarray(x, np.float32)
    nseq = c.shape[0]
    m["cT"] = np.ascontiguousarray(np.asarray(c, np.float32).reshape(nseq, 8, 128).transpose(2, 1, 0)
                                   .reshape(128, 8 * nseq))
    return m


_NC_CACHE = {}


def kernel(**inputs):
    x = np.asarray(inputs["x"], np.float32)
    c = np.asarray(inputs["c"], np.float32)
    B, S, _ = x.shape
    nseq = B // NCORES
    sh = prep_shared(inputs)
    key = (S, nseq)
    if key not in _NC_CACHE:
        _NC_CACHE[key] = build_nc(S, nseq)
    nc = _NC_CACHE[key]
    in_maps = [core_inputs(sh, x[i * nseq:(i + 1) * nseq], c[i * nseq:(i + 1) * nseq]) for i in range(NCORES)]
    res = run_bass_kernel_spmd(nc, in_maps, core_ids=list(range(NCORES)))
    out = np.concatenate([np.asarray(r["out"]) for r in res.results], axis=0)
    return out.astype(np.float32)
```

```python
import numpy as np
from contextlib import ExitStack
import concourse.bass as bass
import concourse.mybir as mybir
from concourse.bass_utils import run_bass_kernel_spmd

F32 = mybir.dt.float32
BF16 = mybir.dt.bfloat16
U32 = mybir.dt.uint32
U8 = mybir.dt.uint8
AF = mybir.ActivationFunctionType
ALU = mybir.AluOpType
AX = mybir.AxisListType

D = 1024
NCORES = 8
EPS = 1e-6
ENGS = ("pe", "act", "dve", "pool", "sp")
ISZ = {F32: 4, BF16: 2, U32: 4, U8: 1}


class Instr:
    __slots__ = ("eng", "idx", "fn", "args", "kwargs", "dma", "deps", "signal",
                 "count", "dsem", "dval")

    def __init__(self, eng, idx, fn, args, kwargs, dma):
        self.eng = eng
        self.idx = idx
        self.fn = fn
        self.args = args
        self.kwargs = kwargs
        self.dma = dma
        self.deps = []
        self.signal = False
        self.count = 0
        self.dsem = None
        self.dval = 0


class Prog:
    def __init__(self, nc, n_dma_sems=40, n_sw=12):
        self.nc = nc
        self.streams = {e: [] for e in ENGS}
        self.reg = {}
        self.barrier = {e: {} for e in ENGS}
        self.n_dma_sems = n_dma_sems
        self.n_sw = n_sw
        self.sw_count = 0
        self.hw_count = 0
        self.dma_last = [None] * n_dma_sems
        self.dma_uses = [0] * n_dma_sems

    def op(self, eng, fn, *args, reads=(), writes=(), dma=False, **kwargs):
        st = self.streams[eng]
        ins = Instr(eng, len(st), fn, args, kwargs, dma)
        deps = {}

        def add(p):
            if p.dma:
                deps[("d", id(p))] = p
            else:
                k = ("e", p.eng)
                q = deps.get(k)
                if q is None or q.idx < p.idx:
                    deps[k] = p

        for r in reads:
            s = self.reg.get(r)
            if s is not None and s[0] is not None:
                add(s[0])
        for w in writes:
            s = self.reg.get(w)
            if s is None:
                continue
            wr = s[0]
            if wr is not None and (wr.dma or dma or wr.eng != eng):
                add(wr)
            for e2, rd in s[1].items():
                if rd.dma or dma or rd.eng != eng:
                    add(rd)
            for rd in s[2]:
                add(rd)
        for p in self.barrier[eng].values():
            if p is not ins:
                add(p)
        self.barrier[eng] = {}
        if dma:
            if eng == "pool":
                slot = self.sw_count % self.n_sw
                self.sw_count += 1
            else:
                slot = self.n_sw + self.hw_count % (self.n_dma_sems - self.n_sw)
                self.hw_count += 1
            prev = self.dma_last[slot]
            if prev is not None:
                deps[("d", id(prev))] = prev
            self.dma_uses[slot] += 1
            ins.dsem = slot
            ins.dval = 16 * self.dma_uses[slot]
            self.dma_last[slot] = ins
        final = []
        for p in deps.values():
            if (not p.dma) and (not dma) and p.eng == eng and eng == "pe":
                continue
            p.signal = True
            final.append(p)
        ins.deps = final
        for r in reads:
            s = self.reg.get(r)
            if s is None:
                s = self.reg[r] = [None, {}, []]
            if dma:
                s[2].append(ins)
            else:
                s[1][eng] = ins
        for w in writes:
            self.reg[w] = [ins, {}, []]
        st.append(ins)
        return ins

    def barrier_all(self, include_sw=False):
        last = {}
        for e in ENGS:
            for q in reversed(self.streams[e]):
                if not q.dma and q.fn is not None:
                    last[e] = q
                    break
        dmas = [d for i_, d in enumerate(self.dma_last) if d is not None and (include_sw or i_ >= self.n_sw)]
        for e in ENGS:
            b = {}
            for e2, p in last.items():
                b[("e", e2)] = p
            for d in dmas:
                b[("d", id(d))] = d
            self.barrier[e] = b

    def emit(self, es):
        nc = self.nc
        self.barrier_all(include_sw=True)
        self.op("sp", None)
        for e in ENGS:
            c = 0
            for ins in self.streams[e]:
                if ins.dma:
                    continue
                if ins.signal:
                    c += 1
                    ins.count = c
        esems = {e: es.enter_context(nc.semaphore("s_" + e)) for e in ENGS}
        dsems = [es.enter_context(nc.semaphore("d%d" % i)) for i in range(self.n_dma_sems)]
        block = es.enter_context(nc.Block())
        handles = {"pe": block.tensor, "act": block.scalar, "dve": block.vector,
                   "pool": block.gpsimd, "sp": block.sync}

        def make(e):
            def body(eng):
                waited = {}
                for ins in self.streams[e]:
                    for p in ins.deps:
                        if p.dma:
                            sem, val, key = dsems[p.dsem], p.dval, ("d", p.dsem)
                        else:
                            sem, val, key = esems[p.eng], p.count, ("e", p.eng)
                        if waited.get(key, 0) >= val:
                            continue
                        waited[key] = val
                        eng.wait_ge(sem, val)
                    if ins.fn is None:
                        continue
                    r = getattr(eng, ins.fn)(*ins.args, **ins.kwargs)
                    if ins.dma:
                        r.then_inc(dsems[ins.dsem], 16)
                    elif ins.signal:
                        r.then_inc(esems[e], 1)
            return body

        for e in ENGS:
            if self.streams[e]:
                handles[e](make(e))


NCONST = 128 + 128 + 1536 + 128 + 8 + 256 + 2048 + 128
C_ID, C_TRI, C_BAND, C_IOTA, C_HM, C_EO, C_SEL, C_NEG = 0, 128, 256, 1792, 1920, 1928, 2184, 4232


def make_consts():
    c = np.zeros((128, NCONST), np.float32)
    c[:, C_ID:C_ID + 128] = np.eye(128)
    s = np.arange(128)
    c[:, C_TRI:C_TRI + 128] = (s[:, None] <= s[None, :])
    band = np.zeros((128, 4, 3, 128), np.float32)
    for g, w in enumerate((2, 4, 8, 16)):
        for t in range(128):
            for ss in range(max(0, t - w + 1), t + 1):
                band[ss, g, 0, t] += 1.0 / w
            band[t, g, 0, t] -= 1.0
            for sp in range(max(0, 128 + t - w + 1), 128):
                band[sp, g, 1, t] += 1.0 / w
            cnt = min(t + 1, w)
            for ss in range(max(0, t - w + 1), t + 1):
                band[ss, g, 2, t] += 1.0 / cnt
            band[t, g, 2, t] -= 1.0
    c[:, C_BAND:C_BAND + 1536] = band.reshape(128, 1536)
    c[:, C_IOTA:C_IOTA + 128] = s[None, :]
    c[:, C_HM:C_HM + 8] = (s[:, None] // 16 == np.arange(8)[None, :])
    eo = np.zeros((128, 2, 128), np.float32)
    eo[:, 0, 0:64] = 1.0
    eo[:, 1, 64:128] = 1.0
    c[:, C_EO:C_EO + 256] = eo.reshape(128, 256)
    sel = np.zeros((128, 16, 128), np.float32)
    for r in range(32):
        sel[r, r % 16, :] = 1.0
    c[:, C_SEL:C_SEL + 2048] = sel.reshape(128, 2048)
    c[:, C_NEG:C_NEG + 128] = np.where(s[:, None] > s[None, :], -30000.0, 0.0)
    return c


def build_nc(S, NSEQ, debug=False):
    NB = S // 128
    NT = S // 256
    PIECE = min(512, S)
    QH = min(1024, S)
    NQH = S // QH
    NPC = QH // PIECE
    TCH = min(512, S)
    NTC = S // TCH

    nc = bass.Bass("TRN2", target_bir_lowering=False)
    dt = nc.dram_tensor

    def din(name, shape, dtype=F32):
        return dt(name, list(shape), dtype, kind="ExternalInput").ap()

    def dscr(name, shape, dtype=BF16):
        return dt(name, list(shape), dtype, kind="Internal").ap()

    x_d = din("x", [NSEQ, S, D])
    cT_d = din("cT", [128, 8 * NSEQ])
    wmod_d = din("wmod", [6, 128, 8 * 1024])
    bmT_d = din("bmT", [128, 48])
    vecT_d = din("vecT", [128, 24])
    fg_d = din("fg", [1, 1024])
    bf_d = din("bf", [1, 16])
    wpairs_d = din("wpairs", [8, 128, 5 * 1024])
    wu_d = din("wu", [128, 4 * 1024])
    wf_d = din("wf", [128, 128])
    wout_d = din("wout", [128, 8 * 1024])
    wq_d = din("wq", [4, 128, 4 * 1024])
    keysT_d = din("keysT", [128, 2048])
    wpool_d = din("wpool", [128, 1024])
    UT_d = din("UT", [128, 128, 1024])
    V_d = din("Vp", [128, 128, 1024])
    consts_d = din("consts", [128, NCONST])
    out_d = dt("out", [NSEQ, S, D], F32, kind="ExternalOutput").ap()

    wpairs_b = dscr("wpairs_b", [8, 128, 5 * 1024])
    wu_b = dscr("wu_b", [128, 4 * 1024])
    wf_b = dscr("wf_b", [128, 128])
    wout_b = dscr("wout_b", [128, 8 * 1024])
    wq_b = dscr("wq_b", [4, 128, 4 * 1024])
    keysT_b = dscr("keysT_b", [128, 2048])
    wpool_b = dscr("wpool_b", [128, 1024])
    UT_b = dscr("UT_b", [128, 128, 1024])
    V_b = dscr("V_b", [128, 128, 1024])

    dbg = {}
    es = ExitStack()
    ARENA = 206 * 1024
    arena = es.enter_context(nc.sbuf_tensor("arena", [128, ARENA], U8))
    banks = [es.enter_context(nc.psum_tensor("psb%d" % i, [128, 512], F32)) for i in range(8)]
    PS = [b[:] for b in banks]
    PSB = [b[:].bitcast(BF16) for b in banks]
    PK = ["ps%d" % i for i in range(8)]
    P = Prog(nc)
    top = [0]

    def alloc(shape, dtype, parts=128):
        n = int(np.prod(shape[1:])) * ISZ[dtype]
        off = (top[0] + 63) // 64 * 64
        top[0] = off + n
        assert top[0] <= ARENA, ("arena overflow", top[0])
        v = arena[:, off:off + n].bitcast(dtype)
        if shape[0] < 128:
            v = v[0:shape[0]]
        if len(shape) == 3:
            v = v.rearrange("p (a b) -> p a b", a=shape[1])
        elif len(shape) == 4:
            v = v.rearrange("p (a b c) -> p a b c", a=shape[1], b=shape[2])
        elif len(shape) == 5:
            v = v.rearrange("p (a b c d) -> p a b c d", a=shape[1], b=shape[2], c=shape[3])
        return v

    def dma(eng, out, in_, reads=(), writes=()):
        return P.op(eng, "dma_start", out=out, in_=in_, reads=reads, writes=writes, dma=True)

    def mm(out, lhsT, rhs, start, stop, reads, writes):
        return P.op("pe", "matmul", out, lhsT=lhsT, rhs=rhs, start=start, stop=stop,
                    reads=reads, writes=writes)

    def tr(out, in_, ident, reads, writes):
        return P.op("pe", "transpose", out=out, in_=in_, identity=ident, reads=reads, writes=writes)

    def act(out, in_, func, reads, writes, **kw):
        return P.op("act", "activation", out=out, in_=in_, func=func, reads=reads, writes=writes, **kw)

    def tt(eng, out, in0, in1, op, reads, writes):
        return P.op(eng, "tensor_tensor", out=out, in0=in0, in1=in1, op=op, reads=reads, writes=writes)

    def ts(eng, out, in0, s1, s2, op0, op1, reads, writes):
        if s2 is None:
            return P.op(eng, "tensor_scalar", out=out, in0=in0, scalar1=s1, scalar2=None, op0=op0,
                        reads=reads, writes=writes)
        return P.op(eng, "tensor_scalar", out=out, in0=in0, scalar1=s1, scalar2=s2, op0=op0, op1=op1,
                    reads=reads, writes=writes)

    def cp(eng, out, in_, reads, writes):
        return P.op(eng, "tensor_copy", out=out, in_=in_, reads=reads, writes=writes)

    def memset(eng, ap, val, writes):
        return P.op(eng, "memset", ap, val, writes=writes)

    ident_bf = alloc([128, 128], BF16)
    ident_f = alloc([128, 128], F32)
    ones_f = alloc([128, 128], F32)
    tri_f = alloc([128, 128], F32)
    tri_bf = alloc([128, 128], BF16)
    neg_bf = alloc([128, 128], BF16)
    band_bf = alloc([128, 4, 3, 128], BF16)
    iota_bf = alloc([128, 128], BF16)
    hmask_bf = alloc([128, 8], BF16)
    keysT_sb = alloc([128, 16, 128], BF16)
    wpool_sb = alloc([128, 4, 256], BF16)
    fg_bc = alloc([128, 1024], F32)
    bf_bc = alloc([128, 16], F32)
    modT = alloc([128, 48, NSEQ], F32)
    vecT = alloc([128, 3, 8], F32)
    bmT = alloc([128, 48], F32)
    cT = alloc([128, 8, NSEQ], F32)
    gs1T = alloc([128, 8, NSEQ], F32)
    gs2T = alloc([128, 8, NSEQ], F32)
    gt1_bc = alloc([128, 1024], F32)
    gt2_bc = alloc([128, 1024], F32)
    yT = alloc([128, 8, S], BF16)
    small = alloc([128, 32], F32)
    persist_top = top[0]

    cst = alloc([128, NCONST], F32)
    dma("sp", cst, consts_d, writes=["cst"])
    dma("sp", vecT, vecT_d.rearrange("p (a b) -> p a b", a=3), writes=["vecT"])
    dma("sp", bmT, bmT_d, writes=["bmT"])
    dma("sp", cT, cT_d.rearrange("p (a b) -> p a b", a=8), writes=["cT"])
    dma("sp", fg_bc, fg_d.partition_broadcast(128), writes=["fg_bc"])
    dma("sp", bf_bc, bf_d.partition_broadcast(128), writes=["bf_bc"])
    dma("pool", wu_b, wu_d, writes=["wu_b"])
    dma("pool", wf_b, wf_d, writes=["wf_b"])
    dma("pool", keysT_b, keysT_d, writes=["keysT_b"])
    dma("pool", wpool_b, wpool_d, writes=["wpool_b"])
    for p in range(8):
        dma("pool", wpairs_b[p], wpairs_d[p], writes=[("wpairs_b", p)])
    dma("pool", wout_b, wout_d, writes=["wout_b"])
    for i in range(4):
        dma("pool", wq_b[i], wq_d[i], writes=[("wq_b", i)])

    cp("dve", ident_bf, cst[:, C_ID:C_ID + 128], ["cst"], ["ident_bf"])
    cp("dve", ident_f, cst[:, C_ID:C_ID + 128], ["cst"], ["ident_f"])
    memset("dve", ones_f, 1.0, ["ones_f"])
    cp("dve", tri_f, cst[:, C_TRI:C_TRI + 128], ["cst"], ["tri_f"])
    cp("dve", tri_bf, cst[:, C_TRI:C_TRI + 128], ["cst"], ["tri_bf"])
    cp("dve", neg_bf, cst[:, C_NEG:C_NEG + 128], ["cst"], ["neg_bf"])
    cp("dve", band_bf.rearrange("p a b c -> p (a b c)"), cst[:, C_BAND:C_BAND + 1536], ["cst"], ["band_bf"])
    cp("dve", iota_bf, cst[:, C_IOTA:C_IOTA + 128], ["cst"], ["iota_bf"])
    cp("dve", hmask_bf, cst[:, C_HM:C_HM + 8], ["cst"], ["hmask_bf"])
    dma("sp", keysT_sb.rearrange("p a b -> p (a b)"), keysT_b, reads=["keysT_b"], writes=["keysT_sb"])
    dma("sp", wpool_sb.rearrange("p a b -> p (a b)"), wpool_b, reads=["wpool_b"], writes=["wpool_sb"])

    wm = [alloc([128, 8, 1024], F32), alloc([128, 8, 1024], F32)]
    for part in range(6):
        w = wm[part % 2]
        wk_ = ("wm", part % 2)
        dma("sp", w.rearrange("p a b -> p (a b)"), wmod_d[part], writes=[wk_])
        pb = 0
        for kc in range(8):
            for dk in range(8):
                mm(PS[pb][:, kc * NSEQ:(kc + 1) * NSEQ], w[:, dk, kc * 128:(kc + 1) * 128], cT[:, dk, :],
                   dk == 0, dk == 7, [wk_, "cT"], [PK[pb]])
        tt("dve", modT[:, part * 8:(part + 1) * 8, :],
           PS[pb][:, 0:8 * NSEQ].rearrange("p (a b) -> p a b", a=8),
           bmT[:, part * 8:(part + 1) * 8].unsqueeze(2).to_broadcast([128, 8, NSEQ]), ALU.add,
           ["bmT"], ["modT", PK[pb]])
    for (gs, gi, sc0) in ((gs1T, 0, 8), (gs2T, 1, 32)):
        ts("dve", gs, modT[:, sc0:sc0 + 8, :], 1.0, None, ALU.add, None, ["modT"], ["gs%d" % gi])
        tt("dve", gs, gs, vecT[:, gi, :].unsqueeze(2).to_broadcast([128, 8, NSEQ]), ALU.mult,
           ["gs%d" % gi, "vecT"], ["gs%d" % gi])
    memset("dve", small, 0.0, ["small"])
    top[0] = persist_top
    P.barrier_all()

    def bcast_gate(b, j0, dst, dkey):
        dg = alloc([128, 8, 128], F32)
        for k in range(8):
            ts("dve", dg[:, k, :], ident_f, modT[:, j0 + k, b:b + 1], None, ALU.mult, None,
               ["ident_f", "modT"], ["dg"])
        for k in range(8):
            pb = 6 + k // 4
            mm(PS[pb][:, (k % 4) * 128:(k % 4 + 1) * 128], ones_f, dg[:, k, :], True, True,
               ["ones_f", "dg"], [PK[pb]])
        cp("dve", dst[:, 0:512], PS[6], [], [dkey, PK[6]])
        cp("dve", dst[:, 512:1024], PS[7], [], [dkey, PK[7]])

    def rstd_of(src, reads, col, junk=None, jkey="junk"):
        if junk is None:
            junk = alloc([128, 1024], BF16)
        memset("dve", small[:, col:col + 1], 0.0, ["small"])
        act(junk, src, AF.Square, reads + ["small"], ["small", jkey], accum_out=small[:, col:col + 1])
        act(small[:, col + 2:col + 3], small[:, col:col + 1], AF.Ln, ["small"], ["small"],
            scale=1.0 / D, bias=EPS)
        act(small[:, col + 1:col + 2], small[:, col + 2:col + 3], AF.Exp, ["small"], ["small"], scale=-0.5)
        return small[:, col + 1:col + 2]

    def norm_to_T(src, skey, dstT, dkey, t0, gsT, shT, b, xs_b, xkey, pb):
        m0 = top[0]
        r = rstd_of(src, [skey], 0)
        ts("dve", xs_b, src, r, None, ALU.mult, None, [skey, "small"], [xkey])
        for k in range(8):
            tr(PSB[pb][:, k * 128:(k + 1) * 128], xs_b[:, k * 128:(k + 1) * 128], ident_bf,
               [xkey, "ident_bf"], [PK[pb]])
        for k in range(8):
            o = dstT[:, k, t0:t0 + 128]
            i = PSB[pb][:, k * 128:(k + 1) * 128]
            if k % 2 == 0:
                act(o, i, AF.Identity, ["gsT", "modT"], [dkey, PK[pb]], scale=gsT[:, k, b:b + 1],
                    bias=shT[:, k, b:b + 1])
            else:
                ts("dve", o, i, gsT[:, k, b:b + 1], shT[:, k, b:b + 1], ALU.mult, ALU.add,
                   ["gsT", "modT"], [dkey, PK[pb]])
        top[0] = m0

    def norm_to_T_multi(blks, dstT, dkey, gsT, shT, b, junk=None, jkey="junk"):
        m0 = top[0]
        if junk is None:
            junk = alloc([128, 1024], BF16)
        n = len(blks)
        for i, (src, skey, t0, xs_b, xkey, pb) in enumerate(blks):
            c0 = 8 + 3 * i
            memset("dve", small[:, c0:c0 + 1], 0.0, [("small", i)])
        for i, (src, skey, t0, xs_b, xkey, pb) in enumerate(blks):
            c0 = 8 + 3 * i
            act(junk, src, AF.Square, [skey, ("small", i)], [("small", i), jkey], accum_out=small[:, c0:c0 + 1])
        for i in range(n):
            c0 = 8 + 3 * i
            act(small[:, c0 + 2:c0 + 3], small[:, c0:c0 + 1], AF.Ln, [("small", i)], [("small", i)],
                scale=1.0 / D, bias=EPS)
        for i in range(n):
            c0 = 8 + 3 * i
            act(small[:, c0 + 1:c0 + 2], small[:, c0 + 2:c0 + 3], AF.Exp, [("small", i)], [("small", i)], scale=-0.5)
        for i, (src, skey, t0, xs_b, xkey, pb) in enumerate(blks):
            c0 = 8 + 3 * i
            ts("dve", xs_b, src, small[:, c0 + 1:c0 + 2], None, ALU.mult, None, [skey, ("small", i)], [xkey])
        for i, (src, skey, t0, xs_b, xkey, pb) in enumerate(blks):
            for k in range(8):
                tr(PSB[pb][:, k * 128:(k + 1) * 128], xs_b[:, k * 128:(k + 1) * 128], ident_bf,
                   [xkey, "ident_bf"], [PK[pb]])
        for i, (src, skey, t0, xs_b, xkey, pb) in enumerate(blks):
            for k in range(8):
                o = dstT[:, k, t0:t0 + 128]
                ii = PSB[pb][:, k * 128:(k + 1) * 128]
                if k % 2 == 0:
                    act(o, ii, AF.Identity, ["gsT", "modT"], [dkey, PK[pb]], scale=gsT[:, k, b:b + 1],
                        bias=shT[:, k, b:b + 1])
                else:
                    ts("dve", o, ii, gsT[:, k, b:b + 1], shT[:, k, b:b + 1], ALU.mult, ALU.add,
                       ["gsT", "modT"], [dkey, PK[pb]])
        top[0] = m0

    for b in range(NSEQ):
        top[0] = persist_top
        P.barrier_all()
        bcast_gate(b, 16, gt1_bc, "gt1_bc")
        bcast_gate(b, 40, gt2_bc, "gt2_bc")
        top[0] = persist_top
        P.barrier_all()
        hT = alloc([128, 8, S], BF16)
        pooledT = alloc([128, 4, S], BF16)
        wpair = [alloc([128, 5, 8, 128], BF16), alloc([128, 5, 8, 128], BF16)]
        Lf = alloc([128, NB, 16], F32)
        Tot = alloc([128, NB, 16], F32)
        carry = alloc([128, NB, 16], F32)
        Csb = alloc([128, NB, 16], F32)
        negCf = alloc([128, NB, 16], F32)
        hif = alloc([128, NB, 16], F32)
        Chl = alloc([128, NB, 32], BF16)
        negC = alloc([32, S], BF16)
        a_top = top[0]
        NXS = min(4, NB)
        xsf = [alloc([128, 1024], F32) for _ in range(NXS)]
        xsb = [alloc([128, 1024], BF16) for _ in range(NXS)]
        u_tm = alloc([128, NB, 512], BF16)
        wu_sb = alloc([128, 4, 8, 128], BF16)
        wf_sb = alloc([128, 8, 16], BF16)
        dma("sp", wu_sb.rearrange("p a b c -> p (a b c)"), wu_b, reads=["wu_b"], writes=["wu_sb"])
        dma("sp", wf_sb.rearrange("p a b -> p (a b)"), wf_b, reads=["wf_b"], writes=["wf_sb"])
        dma("sp", wpair[0].rearrange("p a b c -> p (a b c)"), wpairs_b[0], reads=[("wpairs_b", 0)],
            writes=[("wpair", 0)])
        for j0 in range(0, NB, NXS):
            blks = []
            for sl in range(NXS):
                j = j0 + sl
                dma("sp", xsf[sl], x_d[b, j * 128:(j + 1) * 128, :], writes=[("xsf", sl)])
                blks.append((xsf[sl], ("xsf", sl), j * 128, xsb[sl], ("xsb", sl), sl))
            norm_to_T_multi(blks, hT, "hT", gs1T, modT[:, 0:8, :], b)
        for j in range(NB):
            pb = 2 + j % 2
            for dk in range(8):
                mm(PS[pb].rearrange("p (a b) -> p a b", a=4), hT[:, dk, j * 128:(j + 1) * 128],
                   wu_sb[:, :, dk, :], dk == 0, dk == 7, ["hT", "wu_sb"], [PK[pb]])
            if j % 2 == 0:
                act(u_tm[:, j, :], PS[pb], AF.Copy, [], ["u_tm", PK[pb]])
            else:
                cp("dve", u_tm[:, j, :], PS[pb], [], ["u_tm", PK[pb]])
            for dk in range(8):
                mm(PS[4][:, j * 16:(j + 1) * 16], hT[:, dk, j * 128:(j + 1) * 128], wf_sb[:, dk, :],
                   dk == 0, dk == 7, ["hT", "wf_sb"], [PK[4]])
        tt("dve", Lf, PS[4][:, 0:NB * 16].rearrange("p (a b) -> p a b", a=NB),
           bf_bc.unsqueeze(1).to_broadcast([128, NB, 16]), ALU.add, ["bf_bc"], ["Lf", PK[4]])
        act(Lf, Lf, AF.Exp, ["Lf"], ["Lf"], scale=-1.0)
        act(Lf, Lf, AF.Ln, ["Lf"], ["Lf"], bias=1.0)
        for j in range(NB):
            pb = 2 + j % 2
            for g in range(4):
                o = PS[pb][:, g * 128:(g + 1) * 128]
                mm(o, u_tm[:, j, g * 128:(g + 1) * 128], band_bf[:, g, 0 if j > 0 else 2, :], True, j == 0,
                   ["u_tm", "band_bf"], [PK[pb]])
                if j > 0:
                    mm(o, u_tm[:, j - 1, g * 128:(g + 1) * 128], band_bf[:, g, 1, :], False, True,
                       ["u_tm", "band_bf"], [PK[pb]])
            src = PS[pb].rearrange("p (a b) -> p a b", a=4)
            if j % 2 == 0:
                act(pooledT[:, :, j * 128:(j + 1) * 128], src, AF.Copy, [], ["pooledT", PK[pb]])
            else:
                cp("dve", pooledT[:, :, j * 128:(j + 1) * 128], src, [], ["pooledT", PK[pb]])
        Lflat = Lf.rearrange("p a b -> p (a b)")
        for j in range(NB):
            mm(PS[5][:, j * 16:(j + 1) * 16], tri_f, Lf[:, j, :], True, True, ["tri_f", "Lf"], [PK[5]])
        mm(PS[6][:, 0:NB * 16], ones_f, Lflat, True, True, ["ones_f", "Lf"], [PK[6]])
        cp("dve", Tot.rearrange("p a b -> p (a b)"), PS[6][:, 0:NB * 16], [], ["Tot", PK[6]])
        memset("dve", carry[:, 0, :], 0.0, ["carry"])
        for j in range(1, NB):
            tt("dve", carry[:, j, :], carry[:, j - 1, :], Tot[:, j - 1, :], ALU.add, ["carry", "Tot"], ["carry"])
        tt("dve", Csb.rearrange("p a b -> p (a b)"), PS[5][:, 0:NB * 16], carry.rearrange("p a b -> p (a b)"),
           ALU.add, ["carry"], ["Csb", PK[5]])
        ts("dve", negCf, Csb, -1.0, None, ALU.mult, None, ["Csb"], ["negCf"])
        cp("dve", Chl[:, :, 0:16], negCf, ["negCf"], ["Chl"])
        cp("dve", hif, Chl[:, :, 0:16], ["Chl"], ["hif"])
        tt("dve", Chl[:, :, 16:32], negCf, hif, ALU.subtract, ["negCf", "hif"], ["Chl"])
        for j in range(NB):
            pb = 6 + (j // 8)
            tr(PSB[pb][0:32, (j % 8) * 128:(j % 8 + 1) * 128], Chl[:, j, :], ident_bf, ["Chl", "ident_bf"], [PK[pb]])
        for jb in range((NB + 7) // 8):
            n = min(8, NB - jb * 8) * 128
            cp("dve", negC[:, jb * 1024:jb * 1024 + n], PSB[6 + jb][0:32, 0:n], [], ["negC", PK[6 + jb]])
        if debug and b == 0:
            for nm_, ap_, key_, shp_, dt_ in (("hT", hT, "hT", [128, 8, S], BF16),
                                              ("pooledT", pooledT, "pooledT", [128, 4, S], BF16),
                                              ("Csb", Csb, "Csb", [128, NB, 16], F32)):
                dd = dt("dbg_" + nm_, list(shp_), dt_, kind="ExternalOutput").ap()
                dma("sp", dd, ap_, reads=[key_])
        top[0] = a_top
        P.barrier_all()
        qA = [alloc([128, S], BF16) for _ in range(2)]
        kA = [alloc([128, S], BF16) for _ in range(2)]
        for e_ in range(2):
            memset("dve", kA[e_][64:66, :], 1.0, [("kA", e_)])
        sga = alloc([128, S], BF16)
        sgb = alloc([128, S], BF16)
        vz = alloc([128, NB, 2, 128], BF16)
        PT = [alloc([128, PIECE], BF16) for _ in range(3)]
        rden = alloc([128, PIECE], F32)
        ybn = alloc([128, PIECE], F32)
        memset("pool", vz.rearrange("p a b c -> p (a b c)"), 1.0, ["vz"])
        st_rr = [0]
        for p in range(8):
            wp = wpair[p % 2]
            wpk = ("wpair", p % 2)
            if p + 1 < 8:
                dma("sp", wpair[(p + 1) % 2].rearrange("p a b c -> p (a b c)"), wpairs_b[p + 1],
                    reads=[("wpairs_b", p + 1)], writes=[("wpair", (p + 1) % 2)])
            for e_ in range(2):
                h_ = 2 * p + e_
                dma("sp", qA[e_][64:65, :], negC[h_:h_ + 1, :], reads=["negC"], writes=[("qA", e_)])
                dma("sp", qA[e_][65:66, :], negC[16 + h_:17 + h_, :], reads=["negC"], writes=[("qA", e_)])
            if b == 0:
                for g in range(4 * p, 4 * p + 4):
                    dma("pool", UT_b[g * 4:(g + 1) * 4], UT_d[g * 4:(g + 1) * 4], reads=[("qA", 0)],
                        writes=[("UT_b", g)])
                    dma("pool", V_b[g * 4:(g + 1) * 4], V_d[g * 4:(g + 1) * 4], reads=[("qA", 0)],
                        writes=[("V_b", g)])
            prr = 0
            for wi, which in ((0, "q"), (1, "k"), (3, "ga"), (4, "gb")):
                for tc in range(NTC):
                    pb = 6 + prr % 2
                    prr += 1
                    for dk in range(8):
                        mm(PS[pb][:, 0:TCH], wp[:, wi, dk, :], hT[:, dk, tc * TCH:(tc + 1) * TCH],
                           dk == 0, dk == 7, [wpk, "hT"], [PK[pb]])
                    sl_ = slice(tc * TCH, (tc + 1) * TCH)
                    if which == "q":
                        act(qA[0][0:64, sl_], PS[pb][0:64, 0:TCH], AF.Copy, [], [("qA", 0), PK[pb]], scale=0.125)
                        act(qA[1][0:64, sl_], PS[pb][64:128, 0:TCH], AF.Copy, [], [("qA", 1), PK[pb]], scale=0.125)
                    elif which == "k":
                        cp("dve", kA[0][0:64, sl_], PS[pb][0:64, 0:TCH], [], [("kA", 0), PK[pb]])
                        cp("dve", kA[1][0:64, sl_], PS[pb][64:128, 0:TCH], [], [("kA", 1), PK[pb]])
                    elif which == "ga":
                        act(sga[:, sl_], PS[pb][:, 0:TCH], AF.Sigmoid, [], ["sga", PK[pb]])
                    else:
                        act(sgb[:, sl_], PS[pb][:, 0:TCH], AF.Sigmoid, [], ["sgb", PK[pb]])
            for j0 in range(0, NB, 4):
                nj = min(4, NB - j0)
                pb = 6 + (j0 // 4) % 2
                for jj in range(nj):
                    j = j0 + jj
                    for dk in range(8):
                        mm(PS[pb][:, jj * 128:(jj + 1) * 128], hT[:, dk, j * 128:(j + 1) * 128], wp[:, 2, dk, :],
                           dk == 0, dk == 7, [wpk, "hT"], [PK[pb]])
                src = PS[pb][:, 0:nj * 128].rearrange("p (a b) -> p a b", a=nj)
                act(vz[:, j0:j0 + nj, 0, 0:64], src[:, :, 0:64], AF.Copy, [], ["vz", PK[pb]])
                cp("dve", vz[:, j0:j0 + nj, 1, 64:128], src[:, :, 64:128], [], ["vz", PK[pb]])
            g = p // 2
            half = p % 2
            for tc in range(NTC):
                pb = 6 + tc % 2
                sl_ = slice(tc * TCH, (tc + 1) * TCH)
                mm(PS[pb][:, 0:TCH], wpool_sb[:, g, half * 128:(half + 1) * 128], pooledT[:, g, sl_], True, True,
                   ["wpool_sb", "pooledT"], [PK[pb]])
                P.op("dve", "scalar_tensor_tensor", out=sga[:, sl_], in0=PS[pb][:, 0:TCH], scalar=vecT[:, 2, p:p + 1],
                     in1=sga[:, sl_], op0=ALU.mult, op1=ALU.mult, reads=["vecT", "sga"], writes=["sga", PK[pb]])
            for qh in range(NQH):
                jmax = (qh + 1) * QH // 128
                items = []
                for e in range(2):
                    for j in range(jmax):
                        for pc in range(NPC):
                            ps0 = qh * QH + pc * PIECE
                            ps1 = ps0 + PIECE
                            if ps1 <= j * 128:
                                continue
                            q0 = max(ps0, j * 128)
                            items.append(dict(e=e, j=j, pc=pc, q0=q0, ps1=ps1, n=ps1 - q0, off=q0 - ps0,
                                              diag=(q0 == j * 128), first=(j == 0),
                                              last=(j == min(jmax, ps1 // 128) - 1)))

                def s_stage(it, i):
                    sb_ = 4 + i % 2
                    e = it["e"]
                    j, n, q0, ps1 = it["j"], it["n"], it["q0"], it["ps1"]
                    mm(PS[sb_][:, 0:n], kA[e][0:66, j * 128:(j + 1) * 128], qA[e][0:66, q0:ps1], True, not it["diag"],
                       [("kA", e), ("qA", e)], [PK[sb_]])
                    if it["diag"]:
                        mm(PS[sb_][:, 0:128], ident_bf, neg_bf, False, True, ["ident_bf", "neg_bf"], [PK[sb_]])

                def e_stage(it, i):
                    sb_ = 4 + i % 2
                    pti = i % 3
                    h = 2 * p + it["e"]
                    n = it["n"]
                    act(PT[pti][:, 0:n], PS[sb_][:, 0:n], AF.Exp, ["Csb"], [("PT", pti), PK[sb_]],
                        bias=Csb[:, it["j"], h:h + 1])

                def v_stage(it, i):
                    pti = i % 3
                    n, off, e, j = it["n"], it["off"], it["e"], it["j"]
                    ob = e * NPC + it["pc"]
                    mm(PS[ob][:, off:off + n], vz[:, j, e, :], PT[pti][:, 0:n], it["first"], it["last"],
                       ["vz", ("PT", pti)], [PK[ob]])

                s_stage(items[0], 0)
                for i, it in enumerate(items):
                    if i + 1 < len(items):
                        s_stage(items[i + 1], i + 1)
                    e_stage(it, i)
                    v_stage(it, i)
                for pc in range(NPC):
                    ps0 = qh * QH + pc * PIECE
                    sl_ = slice(ps0, ps0 + PIECE)
                    a0, a1 = PS[pc], PS[NPC + pc]
                    P.op("dve", "reciprocal", out=rden[0:64, :], in_=a0[64:128, 0:PIECE], reads=[],
                         writes=["rden", PK[pc]])
                    P.op("dve", "reciprocal", out=rden[64:128, :], in_=a1[0:64, 0:PIECE], reads=[],
                         writes=["rden", PK[NPC + pc]])
                    tt("dve", ybn[0:64, :], a0[0:64, 0:PIECE], rden[0:64, :], ALU.mult, ["rden"], ["ybn", PK[pc]])
                    tt("dve", ybn[64:128, :], a1[64:128, 0:PIECE], rden[64:128, :], ALU.mult, ["rden"],
                       ["ybn", PK[NPC + pc]])
                    tt("dve", ybn, ybn, sgb[:, sl_], ALU.mult, ["ybn", "sgb"], ["ybn"])
                    tt("dve", yT[:, p, sl_], ybn, sga[:, sl_], ALU.add, ["ybn", "sga"], ["yT"])
        if debug and b == 0:
            dd = dt("dbg_yT", [128, 8, S], BF16, kind="ExternalOutput").ap()
            dma("sp", dd, yT, reads=["yT"])

        top[0] = persist_top
        P.barrier_all()
        GT = alloc([128, 256, 128], BF16)
        _sv = top[0]
        top[0] = ARENA - 16 * 1024 - 64
        wout_sb = alloc([128, 8, 1024], BF16)
        top[0] = _sv
        h2T = alloc([128, 8, 256], BF16)
        xn = alloc([128, 2, 1024], F32)
        gbf = [alloc([128, 16, 8, 16], BF16) for _ in range(2)]
        idxf = [alloc([128, 2, 128], BF16) for _ in range(2)]
        b_top = top[0]
        NRS = 4
        ringU = [alloc([128, 2, 8, 128], BF16) for _ in range(NRS)]
        ringV = [alloc([128, 2, 1024], BF16) for _ in range(NRS)]
        ag = [alloc([128, 256], BF16) for _ in range(3)]
        apb = [alloc([128, 256], BF16) for _ in range(3)]
        tmpo = alloc([128, 512], F32)
        tmpo_j = tmpo.bitcast(BF16)
        xs2 = [alloc([128, 1024], BF16) for _ in range(2)]
        tmpf = [alloc([128, 512], F32) for _ in range(2)]
        b1junk = alloc([128, 1024], BF16)
        assert top[0] <= ARENA - 16 * 1024 - 64, top[0]
        for ti in range(NT):
            tok0 = ti * 256
            if ti == 0:
                dma("sp", wout_sb.rearrange("p a b -> p (a b)"), wout_b, reads=["wout_b"], writes=["wout_sb"])
            for blk in range(2):
                t0 = tok0 + blk * 128
                dma("sp", xn[:, blk, :], x_d[b, t0:t0 + 128, :], writes=[("xn", blk)])
            for blk in range(2):
                t0 = tok0 + blk * 128
                for hf in range(2):
                    pb = blk * 2 + hf
                    for k in range(8):
                        mm(PS[pb], yT[:, k, t0:t0 + 128], wout_sb[:, k, hf * 512:(hf + 1) * 512], k == 0, k == 7,
                           ["yT", "wout_sb"], [PK[pb]])
            for blk in range(2):
                for hf in range(2):
                    pb = blk * 2 + hf
                    tk_ = ("tmpf", hf)
                    tt("dve", tmpf[hf], PS[pb], gt1_bc[:, hf * 512:(hf + 1) * 512], ALU.mult, ["gt1_bc"],
                       [tk_, PK[pb]])
                    tt("pool", xn[:, blk, hf * 512:(hf + 1) * 512], xn[:, blk, hf * 512:(hf + 1) * 512], tmpf[hf],
                       ALU.add, [("xn", blk), tk_], [("xn", blk)])
            norm_to_T_multi([(xn[:, blk, :], ("xn", blk), blk * 128, xs2[blk], ("xs2", blk), 4 + blk)
                             for blk in range(2)], h2T, "h2T", gs2T, modT[:, 24:32, :], b, junk=b1junk, jkey="b1junk")
            if debug and b == 0 and ti == 0:
                dd = dt("dbg_xn", [128, 2, 1024], F32, kind="ExternalOutput").ap()
                dma("sp", dd, xn, reads=[("xn", 0), ("xn", 1)])
                dd = dt("dbg_h2T", [128, 8, 256], BF16, kind="ExternalOutput").ap()
                dma("sp", dd, h2T, reads=["h2T"])
            top[0] = b_top
            P.barrier_all()
            wq_sb = [alloc([128, 4, 8, 128], BF16) for _ in range(2)]
            qpT = alloc([128, 16, 256], BF16)
            sc = alloc([128, 2048], F32)
            wk = alloc([128, 128], F32)
            vl = alloc([128, 16, 16], F32)
            il = alloc([128, 16, 16], U32)
            cand = alloc([128, 8, 256], F32)
            ex = alloc([128, 8, 256], F32)
            cwk = alloc([128, 256], F32)
            cm = alloc([128, 8, 16], F32)
            negm = alloc([128, 8], F32)
            Z = alloc([128, 8], F32)
            for i in range(4):
                dma("sp", wq_sb[i % 2].rearrange("p a b c -> p (a b c)"), wq_b[i], reads=[("wq_b", i)],
                    writes=[("wq_sb", i % 2)])
                for hh in range(4):
                    hp = i * 4 + hh
                    pb = hp % 2
                    for dk in range(8):
                        mm(PS[pb][:, 0:256], wq_sb[i % 2][:, hh, dk, :], h2T[:, dk, :], dk == 0, dk == 7,
                           [("wq_sb", i % 2), "h2T"], [PK[pb]])
                    act(qpT[:, hp, :], PS[pb][:, 0:256], AF.Copy, [], ["qpT", PK[pb]])
            for blk in range(2):
                for hp in range(16):
                    pb = 2 + hp // 4
                    mm(PS[pb][:, (hp % 4) * 128:(hp % 4 + 1) * 128], qpT[:, hp, blk * 128:(blk + 1) * 128],
                       keysT_sb[:, hp, :], True, True, ["qpT", "keysT_sb"], [PK[pb]])
                for q4 in range(4):
                    kk = [("sc", hp_) for hp_ in range(q4 * 4, q4 * 4 + 4)]
                    act(sc[:, q4 * 512:(q4 + 1) * 512], PS[2 + q4], AF.Copy, [], kk + [PK[2 + q4]])
                scs = [sc[:, hp * 128:(hp + 1) * 128] for hp in range(16)]
                for hp in range(16):
                    P.op("dve", "max", out=vl[:, hp, 0:8], in_=scs[hp], reads=[("sc", hp)], writes=[("vl", hp)])
                for hp in range(16):
                    P.op("dve", "max_index", out=il[:, hp, 0:8], in_max=vl[:, hp, 0:8], in_values=scs[hp],
                         reads=[("sc", hp), ("vl", hp)], writes=[("il", hp)])
                for hp in range(16):
                    P.op("dve", "match_replace", out=scs[hp], in_to_replace=vl[:, hp, 0:8], in_values=scs[hp],
                         imm_value=-1e30, reads=[("vl", hp)], writes=[("sc", hp)])
                for hp in range(16):
                    P.op("dve", "max", out=vl[:, hp, 8:16], in_=scs[hp], reads=[("sc", hp)], writes=[("vl", hp)])
                for hp in range(16):
                    P.op("dve", "max_index", out=il[:, hp, 8:16], in_max=vl[:, hp, 8:16], in_values=scs[hp],
                         reads=[("sc", hp), ("vl", hp)], writes=[("il", hp)])
                vl4 = vl.rearrange("q (h p) a -> q h p a", p=2)
                cand4 = cand.rearrange("q h (a b) -> q h a b", a=16)
                tt("dve", cand4, vl4[:, :, 0, :].unsqueeze(3).to_broadcast([128, 8, 16, 16]),
                   vl4[:, :, 1, :].unsqueeze(2).to_broadcast([128, 8, 16, 16]), ALU.add,
                   [("vl", i_) for i_ in range(16)], ["cand"])
                for h in range(8):
                    P.op("dve", "max", out=cm[:, h, 0:8], in_=cand[:, h, :], reads=["cand"], writes=[("cm", h)])
                for h in range(8):
                    P.op("dve", "match_replace", out=ex[:, h, :], in_to_replace=cm[:, h, 0:8], in_values=cand[:, h, :],
                         imm_value=-1e30, reads=["cand", ("cm", h)], writes=[("ex", h)])
                for h in range(8):
                    P.op("dve", "max", out=cm[:, h, 8:16], in_=ex[:, h, :], reads=[("ex", h)], writes=[("cm", h)])
                ts("dve", negm, cm[:, :, 0], -1.0, None, ALU.mult, None, [("cm", i_) for i_ in range(8)], ["negm"])
                for h in range(8):
                    act(ex[:, h, :], cand[:, h, :], AF.Exp, ["cand", "negm"], [("ex", h)], bias=negm[:, h:h + 1])
                for h in range(8):
                    P.op("dve", "scalar_tensor_tensor", out=ex[:, h, :], in0=cand[:, h, :], scalar=cm[:, h, 15:16],
                         in1=ex[:, h, :], op0=ALU.is_ge, op1=ALU.mult, reads=["cand", ("cm", h), ("ex", h)],
                         writes=[("ex", h)])
                exk = [("ex", i_) for i_ in range(8)]
                P.op("dve", "tensor_reduce", out=Z, in_=ex, axis=AX.X, op=ALU.add, reads=exk, writes=["Z"])
                P.op("dve", "reciprocal", out=Z, in_=Z, reads=["Z"], writes=["Z"])
                tt("dve", gbf[blk].rearrange("q a h b -> q h a b"), ex.rearrange("q h (a b) -> q h a b", a=16),
                   Z.unsqueeze(2).unsqueeze(3).to_broadcast([128, 8, 16, 16]), ALU.mult, exk + ["Z"], [("gbf", blk)])
                il4 = il.rearrange("q (h p) a -> q h p a", p=2)
                for pp in range(2):
                    cp("dve", idxf[blk][:, pp, :].rearrange("q (h a) -> q h a", h=8), il4[:, :, pp, :],
                       [("il", i_) for i_ in range(16)], [("idxf", blk)])
            top[0] = b_top
            P.barrier_all()
            idxT = alloc([128, 2, 128], BF16)
            gbdf = alloc([128, 8, 16, 128], BF16)
            hmask_f = alloc([128, 8], F32)
            cp("dve", hmask_f, hmask_bf, ["hmask_bf"], ["hmask_f"])
            NSL = 3
            P1 = [alloc([128, 16, 128], BF16) for _ in range(NSL)]
            P2 = [alloc([128, 16, 128], BF16) for _ in range(NSL)]
            Msb = [alloc([128, 4, 128], BF16) for _ in range(3)]
            MB = (3, 4, 7)
            for blk in range(2):
                for pp in range(2):
                    tr(PSB[0][:, pp * 128:(pp + 1) * 128], idxf[blk][:, pp, :], ident_bf,
                       [("idxf", blk), "ident_bf"], [PK[0]])
                cp("dve", idxT.rearrange("p a b -> p (a b)"), PSB[0][:, 0:256], [], ["idxT", PK[0]])
                for a in range(16):
                    pb = 1 + a // 8
                    tr(PSB[pb][:, (a % 8) * 128:(a % 8 + 1) * 128], gbf[blk][:, a, :, :].rearrange("q h b -> q (h b)"),
                       ident_bf, [("gbf", blk), "ident_bf"], [PK[pb]])
                for hh in range(8):
                    for half in range(2):
                        act(gbdf[:, hh, half * 8:(half + 1) * 8, :].rearrange("p a t -> p (a t)"), PSB[1 + half],
                            AF.Copy, ["hmask_f"], ["gbdf", PK[1 + half]], scale=hmask_f[:, hh:hh + 1])

                def build(sub):
                    sl = sub % NSL
                    t16 = sub * 16
                    tt("dve", P1[sl], iota_bf.unsqueeze(1).to_broadcast([128, 16, 128]),
                       idxT[:, 0, t16:t16 + 16].unsqueeze(2).to_broadcast([128, 16, 128]), ALU.is_equal,
                       ["iota_bf", "idxT"], [("P1", sl)])
                    tt("dve", P2[sl], iota_bf.unsqueeze(1).to_broadcast([128, 16, 128]),
                       idxT[:, 1, t16:t16 + 16].unsqueeze(2).to_broadcast([128, 16, 128]), ALU.is_equal,
                       ["iota_bf", "idxT"], [("P2", sl)])

                def m_stage(q):
                    sl = (q // 4) % NSL
                    pbm = MB[q % 3]
                    for tq in range(4):
                        t = (q % 4) * 4 + tq
                        mm(PS[pbm][:, tq * 128:(tq + 1) * 128], gbdf[:, :, :, (q // 4) * 16 + t], P2[sl][:, t, :], True, True,
                           ["gbdf", ("P2", sl)], [PK[pbm]])

                def me_stage(q):
                    pbm = MB[q % 3]
                    ms = q % 3
                    act(Msb[ms].rearrange("p a b -> p (a b)"), PS[pbm], AF.Copy, [], [("Msb", ms), PK[pbm]])

                def g_stage(q):
                    sl = (q // 4) % NSL
                    ms = q % 3
                    pbg = 5 + q % 2
                    for tq in range(4):
                        t = (q % 4) * 4 + tq
                        mm(PS[pbg][:, tq * 128:(tq + 1) * 128], P1[sl][:, t, :], Msb[ms][:, tq, :], True, True,
                           [("P1", sl), ("Msb", ms)], [PK[pbg]])

                def ge_stage(q):
                    pbg = 5 + q % 2
                    tk = blk * 128 + q * 4
                    dst = GT[:, tk:tk + 4, :].rearrange("p t c -> p (t c)")
                    act(dst, PS[pbg], AF.Copy, [], ["GT", PK[pbg]])

                NQ = 32
                build(0)
                build(1)
                m_stage(0)
                m_stage(1)
                me_stage(0)
                for q in range(NQ):
                    if q % 4 == 0 and q // 4 + 2 < 8:
                        build(q // 4 + 2)
                    if q + 2 < NQ:
                        m_stage(q + 2)
                    if q + 1 < NQ:
                        me_stage(q + 1)
                    g_stage(q)
                    ge_stage(q)
            if debug and b == 0 and ti == 0:
                dd = dt("dbg_GT", [128, 256, 128], BF16, kind="ExternalOutput").ap()
                dma("sp", dd, GT, reads=["GT"])
            top[0] = b_top
            P.barrier_all()

            def load_group(g):
                sl = g % NRS
                dma("sp", ringU[sl].rearrange("p c k e -> p c (k e)"),
                    UT_b[g * 2:(g + 1) * 2].rearrange("c p f -> p c f"),
                    reads=[("UT_b", g // 2)], writes=[("ringU", sl)])
                dma("sp", ringV[sl], V_b[g * 2:(g + 1) * 2].rearrange("c p f -> p c f"),
                    reads=[("V_b", g // 2)], writes=[("ringV", sl)])

            def mm1(c):
                sl = (c // 2) % NRS
                cc = c % 2
                pb = 4 + c % 4
                for dk in range(8):
                    mm(PS[pb][:, 0:256], ringU[sl][:, cc, dk, :], h2T[:, dk, :], dk == 0, dk == 7,
                       [("ringU", sl), "h2T"], [PK[pb]])

            def mid(c):
                pb = 4 + c % 4
                i = c % 3
                act(ag[i], PS[pb][:, 0:256], AF.Gelu, [], [("ag", i), PK[pb]])
                tt("dve", apb[i], ag[i], GT[:, :, c], ALU.mult, [("ag", i), "GT"], [("apb", i)])

            def mm2(c):
                sl = (c // 2) % NRS
                cc = c % 2
                i = c % 3
                for th in range(2):
                    for dh in range(2):
                        pb = th * 2 + dh
                        mm(PS[pb], apb[i][:, th * 128:(th + 1) * 128], ringV[sl][:, cc, dh * 512:(dh + 1) * 512],
                           c == 0, c == 127, [("apb", i), ("ringV", sl)], [PK[pb]])

            for g_ in range(NRS):
                load_group(g_)
            if ti + 1 < NT:
                dma("sp", wout_sb.rearrange("p a b -> p (a b)"), wout_b, reads=["wout_b"], writes=["wout_sb"])
            mm1(0)
            mm1(1)
            for c in range(128):
                if c + 2 < 128:
                    mm1(c + 2)
                mid(c)
                mm2(c)
                if c % 2 == 1 and c // 2 + NRS < 64:
                    load_group(c // 2 + NRS)
            for th in range(2):
                for dh in range(2):
                    pb = th * 2 + dh
                    tt("dve", tmpo, PS[pb], gt2_bc[:, dh * 512:(dh + 1) * 512], ALU.mult, ["gt2_bc"],
                       ["tmpo", PK[pb]])
                    tt("dve", xn[:, th, dh * 512:(dh + 1) * 512], xn[:, th, dh * 512:(dh + 1) * 512], tmpo,
                       ALU.add, [("xn", th), "tmpo"], [("xn", th)])
                r = rstd_of(xn[:, th, :], [("xn", th)], 4, junk=tmpo_j, jkey="tmpo")
                P.op("dve", "scalar_tensor_tensor", out=xn[:, th, :], in0=xn[:, th, :], scalar=r, in1=fg_bc,
                     op0=ALU.mult, op1=ALU.mult, reads=[("xn", th), "small", "fg_bc"], writes=[("xn", th)])
                t0 = tok0 + th * 128
                dma("sp", out_d[b, t0:t0 + 128, :], xn[:, th, :], reads=[("xn", th)])
    P.emit(es)
    es.close()
    return nc


def blk(w):
    n = w.shape[1]
    return np.ascontiguousarray(w.reshape(8, 128, n).transpose(1, 0, 2))


def prep_shared(inp):
    f = np.float32
    w_in = np.asarray(inp["w_in"][0], f)
    sh = {}
    wp = []
    for p in range(8):
        cols = [512 + p * 128, 1536 + p * 128, 2560 + p * 128, 3600 + p * 128, 4624 + p * 128]
        wp.append(np.stack([blk(w_in[:, c:c + 128]) for c in cols], axis=1))
    sh["wpairs"] = np.ascontiguousarray(np.stack(wp, 0).reshape(8, 128, 5 * 1024))
    sh["wu"] = np.ascontiguousarray(
        np.stack([blk(w_in[:, c * 128:(c + 1) * 128]) for c in range(4)], axis=1).reshape(128, 4 * 1024))
    sh["wf"] = np.ascontiguousarray(blk(w_in[:, 3584:3600]).reshape(128, 128))
    sh["wout"] = np.ascontiguousarray(blk(np.asarray(inp["w_out"][0], f)).reshape(128, 8 * 1024))
    wq = np.asarray(inp["peer_w_query"][0], f)
    wqb = np.stack([blk(wq[:, c * 128:(c + 1) * 128]) for c in range(16)], axis=0)
    sh["wq"] = np.ascontiguousarray(wqb.reshape(4, 4, 128, 1024).transpose(0, 2, 1, 3).reshape(4, 128, 4096))
    keys = np.asarray(inp["peer_sub_keys"][0], f)
    sh["keysT"] = np.ascontiguousarray(keys.transpose(3, 0, 1, 2).reshape(128, 2048))
    sh["wpool"] = np.ascontiguousarray(np.asarray(inp["w_pool"][0], f).transpose(1, 0, 2).reshape(128, 1024))
    U = np.asarray(inp["peer_u"][0], f).reshape(128, 128, 8, 128)
    sh["UT"] = np.ascontiguousarray(U.transpose(1, 3, 2, 0).reshape(128, 128, 1024))
    V = np.asarray(inp["peer_v"][0], f).reshape(128, 128, 1024)
    sh["Vp"] = np.ascontiguousarray(V.transpose(1, 0, 2))
    wm = np.asarray(inp["w_mod"][0], f)
    sh["wmod"] = np.ascontiguousarray(
        np.stack([blk(wm[:, q * 1024:(q + 1) * 1024]) for q in range(6)], 0).reshape(6, 128, 8192))
    sh["bmT"] = np.ascontiguousarray(np.asarray(inp["b_mod"][0], f).reshape(48, 128).T)
    vec = np.stack([np.asarray(inp["norm1_g"][0], f).reshape(8, 128).T,
                    np.asarray(inp["norm2_g"][0], f).reshape(8, 128).T,
                    np.asarray(inp["pool_scale"][0], f).reshape(8, 128).T], axis=1)
    sh["vecT"] = np.ascontiguousarray(vec.reshape(128, 24))
    sh["fg"] = np.ascontiguousarray(np.asarray(inp["final_g"], f).reshape(1, 1024))
    sh["bf"] = np.ascontiguousarray(np.asarray(inp["b_f"][0], f).reshape(1, 16))
    sh["consts"] = make_consts()
    return sh


def core_inputs(sh, x, c):
    m = dict(sh)
    m["x"] = np.ascontiguousarray(x, np.float32)
    nseq = c.shape[0]
    m["cT"] = np.ascontiguousarray(np.asarray(c, np.float32).reshape(nseq, 8, 128).transpose(2, 1, 0)
                                   .reshape(128, 8 * nseq))
    return m


_NC_CACHE = {}


def kernel(**inputs):
    x = np.asarray(inputs["x"], np.float32)
    c = np.asarray(inputs["c"], np.float32)
    B, S, _ = x.shape
    nseq = B // NCORES
    sh = prep_shared(inputs)
    key = (S, nseq)
    if key not in _NC_CACHE:
        _NC_CACHE[key] = build_nc(S, nseq)
    nc = _NC_CACHE[key]
    in_maps = [core_inputs(sh, x[i * nseq:(i + 1) * nseq], c[i * nseq:(i + 1) * nseq]) for i in range(NCORES)]
    res = run_bass_kernel_spmd(nc, in_maps, core_ids=list(range(NCORES)))
    out = np.concatenate([np.asarray(r["out"]) for r in res.results], axis=0)
    return out.astype(np.float32)
```
